# Optimizing a Trainium2 kernel written in Bass

```python
import jax, jax.numpy as jnp
from jax import lax
import numpy as np

D_MODEL = 1024
BATCH = 8
SEQ = 4096
DEPTH = 4

CTX_LEN = 256
GRID_W = 64
N_EVEN = (DEPTH + 1) // 2
N_ODD = DEPTH // 2
EPS = 1e-6

M_HEADS = 4
M_HEAD_DIM = D_MODEL // M_HEADS
M_WIDTH = M_HEADS * M_HEAD_DIM
M_CHUNK = 128
M_CONV = 3
P_WINDOWS = (2, 4, 8, 16)
P_GROUPS = len(P_WINDOWS)
P_GROUP_DIM = D_MODEL // P_GROUPS
P_WIDTH = P_GROUPS * P_GROUP_DIM
AB_WIDTH = M_WIDTH + P_WIDTH
N_GATE = 4 * M_HEADS
GATE_OFF = 5 * M_WIDTH + 2 * P_WIDTH
AB_IN = GATE_OFF + N_GATE

A_HEADS = 16
A_KV_HEADS = 4
A_GROUP = A_HEADS // A_KV_HEADS
A_HEAD_DIM = D_MODEL // A_HEADS
A_WIDTH = A_HEADS * A_HEAD_DIM
A_KV_WIDTH = A_KV_HEADS * A_HEAD_DIM
A_WINDOW = 128
A_BLOCK = 128
A_BAND = A_BLOCK + 2 * A_WINDOW
ROPE_BASE = 10000.0
C_IN = 2 * A_WIDTH + 2 * A_KV_WIDTH

kernel_name = 'hybrid_mlstm_pool_swa_diffusion_trunk'

f32 = jnp.float32


def rmsnorm(x, g):
    xf = x.astype(f32)
    return (xf * lax.rsqrt(jnp.mean(xf * xf, -1, keepdims=True) + EPS) * g).astype(x.dtype)


def short_conv(u, w):
    pad = w.shape[0] // 2
    return lax.conv_general_dilated(u, w[:, None, :].astype(u.dtype), (1,), [(pad, pad)],
                                    dimension_numbers=('NWC', 'WIO', 'NWC'),
                                    feature_group_count=u.shape[-1])


def to_heads(a):
    bsz, n, _ = a.shape
    return a.reshape(bsz, n, M_HEADS, M_HEAD_DIM).transpose(0, 2, 1, 3).astype(f32)


def mlstm_chunkwise(q, k, v, log_i, log_f, state):
    bsz, nh, n, dh = q.shape
    nc = n // M_CHUNK

    def chunks(a):
        return jnp.moveaxis(a.reshape(a.shape[:2] + (nc, M_CHUNK) + a.shape[3:]), 2, 0)

    lower = jnp.tril(jnp.ones((M_CHUNK, M_CHUNK), bool))

    def step(carry, inp):
        c_mem, n_mem, m_prev = carry
        qc, kc, vc, lic, lfc = inp
        b = jnp.cumsum(lfc, axis=-1)
        dmat = b[..., :, None] - b[..., None, :] + lic[..., None, :]
        dmat = jnp.where(lower, dmat, -jnp.inf)
        inter = b + m_prev[..., None]
        m_t = jnp.maximum(inter, dmat.max(-1))
        w_inter = jnp.exp(inter - m_t)
        s = jnp.einsum('bhtd,bhsd->bhts', qc, kc) * jnp.exp(dmat - m_t[..., None])
        num = w_inter[..., None] * jnp.einsum('bhtd,bhde->bhte', qc, c_mem) + jnp.einsum('bhts,bhse->bhte', s, vc)
        den = w_inter * jnp.einsum('bhtd,bhd->bht', qc, n_mem) + s.sum(-1)
        h = num / jnp.maximum(jnp.abs(den), jnp.exp(-m_t))[..., None]
        b_last = b[..., -1]
        d_last = b_last[..., None] - b + lic
        m_new = jnp.maximum(b_last + m_prev, d_last.max(-1))
        w_s = jnp.exp(d_last - m_new[..., None])[..., None] * kc
        decay = jnp.exp(b_last + m_prev - m_new)
        c_new = decay[..., None, None] * c_mem + jnp.einsum('bhsd,bhse->bhde', w_s, vc)
        n_new = decay[..., None] * n_mem + w_s.sum(2)
        return (c_new, n_new, m_new), h

    state, hs = lax.scan(step, state, (chunks(q), chunks(k), chunks(v), chunks(log_i), chunks(log_f)))
    return jnp.moveaxis(hs, 0, 2).reshape(bsz, nh, n, dh), state


def mlstm_inputs(u, conv_w, b_gate):
    bsz, n, _ = u.shape
    qk = short_conv(u[..., :2 * M_WIDTH], conv_w)
    q = to_heads(qk[..., :M_WIDTH])
    k = to_heads(qk[..., M_WIDTH:]) * (M_HEAD_DIM ** -0.5)
    v = to_heads(u[..., 2 * M_WIDTH:3 * M_WIDTH])
    g = (u[..., GATE_OFF:] + b_gate).astype(f32).reshape(bsz, n, 4, M_HEADS)
    g = jnp.transpose(g, (2, 0, 3, 1))
    return q, k, v, (g[0], jax.nn.log_sigmoid(g[1]), g[2], jax.nn.log_sigmoid(g[3]))


def multiscale_pool(u, pool_w, pool_scale):
    bsz, n, _ = u.shape
    ug = u.astype(f32).reshape(bsz, n, P_GROUPS, P_GROUP_DIM)
    csum = jnp.concatenate([jnp.zeros_like(ug[:, :1]), jnp.cumsum(ug, axis=1)], axis=1)
    t = jnp.arange(n)[:, None]
    half = jnp.array(P_WINDOWS, jnp.int32)[None, :] // 2
    lo = jnp.clip(t - half, 0, n)
    hi = jnp.clip(t + half, 0, n)
    gi = jnp.arange(P_GROUPS)[None, :]
    win_sum = csum[:, hi, gi] - csum[:, lo, gi]
    mean = win_sum / (hi - lo).astype(f32)[None, :, :, None]
    mixed = jnp.einsum('bngc,gcd->bngd', mean - ug, pool_w.astype(f32))
    return mixed.reshape(bsz, n, P_WIDTH) * pool_scale


def head_rmsnorm(h, w):
    bsz, n, _ = h.shape
    hh = h.reshape(bsz, n, M_HEADS, M_HEAD_DIM)
    hh = hh * lax.rsqrt(jnp.mean(hh * hh, -1, keepdims=True) + EPS)
    return hh.reshape(bsz, n, M_WIDTH) * w


def mlstm_pool_output(u, h, mnorm_w, pool_w, pool_scale, w_out):
    bsz, n, _ = u.shape
    o_gate = jax.nn.sigmoid(u[..., 3 * M_WIDTH:4 * M_WIDTH].astype(f32))
    h = jnp.transpose(h, (0, 2, 1, 3)).reshape(bsz, n, M_WIDTH) * o_gate
    y_m = head_rmsnorm(h, mnorm_w) * jax.nn.silu(u[..., 4 * M_WIDTH:5 * M_WIDTH].astype(f32))
    p0 = 5 * M_WIDTH
    y_p = multiscale_pool(u[..., p0:p0 + P_WIDTH], pool_w, pool_scale) * jax.nn.silu(
        u[..., p0 + P_WIDTH:p0 + 2 * P_WIDTH].astype(f32))
    return jnp.concatenate([y_m, y_p], -1).astype(u.dtype) @ w_out


def mlstm_pool_mixer(hx, hc, w_in, b_gate, conv_w, mnorm_w, pool_w, pool_scale, w_out, need_ctx):
    ux, uc = hx @ w_in, hc @ w_in
    qx, kx, vx, (lif_x, lff_x, lib_x, lfb_x) = mlstm_inputs(ux, conv_w, b_gate)
    qc, kc, vc, (lif_c, lff_c, lib_c, lfb_c) = mlstm_inputs(uc, conv_w, b_gate)
    bsz = hx.shape[0]
    zero = (jnp.zeros((bsz, M_HEADS, M_HEAD_DIM, M_HEAD_DIM), f32),
            jnp.zeros((bsz, M_HEADS, M_HEAD_DIM), f32),
            jnp.zeros((bsz, M_HEADS), f32))
    rev = lambda a: jnp.flip(a, axis=2)
    hc_f, st_f = mlstm_chunkwise(qc, kc, vc, lif_c, lff_c, zero)
    hx_f, _ = mlstm_chunkwise(qx, kx, vx, lif_x, lff_x, st_f)
    hc_b, st_b = mlstm_chunkwise(rev(qc), rev(kc), rev(vc), rev(lib_c), rev(lfb_c), zero)
    hx_b, _ = mlstm_chunkwise(rev(qx), rev(kx), rev(vx), rev(lib_x), rev(lfb_x), st_b)
    yx = mlstm_pool_output(ux, hx_f + rev(hx_b), mnorm_w, pool_w, pool_scale, w_out)
    yc = mlstm_pool_output(uc, hc_f + rev(hc_b), mnorm_w, pool_w, pool_scale, w_out) if need_ctx else None
    return yx, yc


def axial_rope(n):
    rows = n // GRID_W
    row = jnp.repeat(jnp.arange(rows), GRID_W).astype(f32)
    col = jnp.tile(jnp.arange(GRID_W), rows).astype(f32)
    n_freq = A_HEAD_DIM // 4
    inv = ROPE_BASE ** (-jnp.arange(n_freq, dtype=f32) / n_freq)
    ang = jnp.concatenate([row[:, None] * inv, col[:, None] * inv], -1)
    return jnp.cos(ang), jnp.sin(ang)


def apply_rope(x, cos, sin):
    xf = x.astype(f32)
    x1, x2 = xf[..., 0::2], xf[..., 1::2]
    c, s = cos[None, :, None, :], sin[None, :, None, :]
    return jnp.stack([x1 * c - x2 * s, x1 * s + x2 * c], -1).reshape(x.shape).astype(x.dtype)


def gqa_scores(q, k):
    return jnp.einsum('bqkgd,bskd->bkgqs', q, k).astype(f32) * (A_HEAD_DIM ** -0.5)


def window_attention(q, k, v, k_ctx, v_ctx, sink):
    bsz, n = q.shape[:2]
    n_ctx = k_ctx.shape[1]
    pad = ((0, 0), (A_WINDOW, A_WINDOW), (0, 0), (0, 0))
    kp, vp = jnp.pad(k, pad), jnp.pad(v, pad)
    qg = q.reshape(bsz, n, A_KV_HEADS, A_GROUP, A_HEAD_DIM)
    sink_l = sink.reshape(1, A_KV_HEADS, A_GROUP, 1, 1).astype(f32)

    def block(bi):
        start = bi * A_BLOCK
        qb = lax.dynamic_slice_in_dim(qg, start, A_BLOCK, axis=1)
        kb = lax.dynamic_slice_in_dim(kp, start, A_BAND, axis=1)
        vb = lax.dynamic_slice_in_dim(vp, start, A_BAND, axis=1)
        qi = start + jnp.arange(A_BLOCK)
        kj = start - A_WINDOW + jnp.arange(A_BAND)
        ok = (jnp.abs(qi[:, None] - kj[None, :]) <= A_WINDOW) & (kj >= 0) & (kj < n)
        s_lat = jnp.where(ok, gqa_scores(qb, kb), -jnp.inf)
        s_ctx = gqa_scores(qb, k_ctx)
        sk = jnp.broadcast_to(sink_l, s_ctx.shape[:-1] + (1,))
        p = jax.nn.softmax(jnp.concatenate([s_lat, s_ctx, sk], -1), axis=-1)
        o = (jnp.einsum('bkgqs,bskd->bqkgd', p[..., :A_BAND].astype(v.dtype), vb)
             + jnp.einsum('bkgqs,bskd->bqkgd', p[..., A_BAND:A_BAND + n_ctx].astype(v.dtype), v_ctx))
        return o.reshape(bsz, A_BLOCK, A_WIDTH)

    out = lax.map(block, jnp.arange(n // A_BLOCK))
    return jnp.moveaxis(out, 0, 1).reshape(bsz, n, A_WIDTH)


def context_attention(q, k, v, sink):
    bsz, n = q.shape[:2]
    s = gqa_scores(q.reshape(bsz, n, A_KV_HEADS, A_GROUP, A_HEAD_DIM), k)
    sk = jnp.broadcast_to(sink.reshape(1, A_KV_HEADS, A_GROUP, 1, 1).astype(f32), s.shape[:-1] + (1,))
    p = jax.nn.softmax(jnp.concatenate([s, sk], -1), axis=-1)
    o = jnp.einsum('bkgqs,bskd->bqkgd', p[..., :n].astype(v.dtype), v)
    return o.reshape(bsz, n, A_WIDTH)


def split_attn(u):
    bsz, n, _ = u.shape
    q = u[..., :A_WIDTH].reshape(bsz, n, A_HEADS, A_HEAD_DIM)
    k = u[..., A_WIDTH:A_WIDTH + A_KV_WIDTH].reshape(bsz, n, A_KV_HEADS, A_HEAD_DIM)
    v = u[..., A_WIDTH + A_KV_WIDTH:A_WIDTH + 2 * A_KV_WIDTH].reshape(bsz, n, A_KV_HEADS, A_HEAD_DIM)
    z = u[..., A_WIDTH + 2 * A_KV_WIDTH:]
    return q, k, v, z


def window_attention_mixer(hx, hc, w_in, sink, w_out, need_ctx):
    qx, kx, vx, zx = split_attn(hx @ w_in)
    qc, kc, vc, zc = split_attn(hc @ w_in)
    cos, sin = axial_rope(hx.shape[1])
    qx, kx = apply_rope(qx, cos, sin), apply_rope(kx, cos, sin)
    ox = window_attention(qx, kx, vx, kc, vc, sink)
    yx = (ox * jax.nn.silu(zx)) @ w_out
    yc = (context_attention(qc, kc, vc, sink) * jax.nn.silu(zc)) @ w_out if need_ctx else None
    return yx, yc


def setup_inputs(seed: int = 0) -> dict:
    key = jax.random.key(seed)
    ks = jax.random.split(key, 20)
    nrm = lambda k, shape, s: jax.random.normal(k, shape, f32) * s
    kg = jax.random.split(ks[9], 2)
    i_bias = nrm(kg[0], (N_EVEN, 2, M_HEADS), 0.1)
    f_bias = jnp.linspace(3.0, 6.0, M_HEADS, dtype=f32) + nrm(kg[1], (N_EVEN, 2, M_HEADS), 0.1)
    return {
        'x': nrm(ks[0], (BATCH, SEQ, D_MODEL), 1.0),
        'c': nrm(ks[1], (BATCH, D_MODEL), 1.0),
        'ctx': nrm(ks[2], (BATCH, CTX_LEN, D_MODEL), 1.0),
        'c_ctx': nrm(ks[3], (D_MODEL,), 1.0),
        'w_mod': nrm(ks[4], (DEPTH, D_MODEL, 3 * D_MODEL), 0.5 * D_MODEL ** -0.5),
        'b_mod': nrm(ks[5], (DEPTH, 3 * D_MODEL), 0.01),
        'g_pre': 1.0 + nrm(ks[6], (DEPTH, D_MODEL), 0.02),
        'g_post': 1.0 + nrm(ks[7], (DEPTH, D_MODEL), 0.02),
        'ab_w_in': nrm(ks[8], (N_EVEN, D_MODEL, AB_IN), D_MODEL ** -0.5),
        'ab_b_gate': jnp.stack([i_bias, f_bias], axis=2).reshape(N_EVEN, N_GATE),
        'ab_conv': nrm(ks[10], (N_EVEN, M_CONV, 2 * M_WIDTH), M_CONV ** -0.5),
        'ab_mnorm': 1.0 + nrm(ks[11], (N_EVEN, M_WIDTH), 0.02),
        'ab_pool_w': nrm(ks[12], (N_EVEN, P_GROUPS, P_GROUP_DIM, P_GROUP_DIM), P_GROUP_DIM ** -0.5),
        'ab_pool_scale': 1.0 + nrm(ks[13], (N_EVEN, P_WIDTH), 0.02),
        'ab_w_out': nrm(ks[14], (N_EVEN, AB_WIDTH, D_MODEL), AB_WIDTH ** -0.5),
        'c_w_in': nrm(ks[15], (N_ODD, D_MODEL, C_IN), D_MODEL ** -0.5),
        'c_sink': nrm(ks[16], (N_ODD, A_HEADS), 1.0),
        'c_w_out': nrm(ks[17], (N_ODD, A_WIDTH, D_MODEL), A_WIDTH ** -0.5),
    }


def reference(x, c, ctx, c_ctx, w_mod, b_mod, g_pre, g_post, ab_w_in, ab_b_gate, ab_conv, ab_mnorm,
              ab_pool_w, ab_pool_scale, ab_w_out, c_w_in, c_sink, c_w_out):
    for l in range(DEPTH):
        j = l // 2
        last = l == DEPTH - 1
        shift, scale, gate = jnp.split(jax.nn.silu(c) @ w_mod[l] + b_mod[l], 3, axis=-1)
        shift_c, scale_c, gate_c = jnp.split(jax.nn.silu(c_ctx) @ w_mod[l] + b_mod[l], 3, axis=-1)
        hx = rmsnorm(x, g_pre[l]) * (1 + scale[:, None]) + shift[:, None]
        hc = rmsnorm(ctx, g_pre[l]) * (1 + scale_c) + shift_c
        if l % 2 == 0:
            yx, yc = mlstm_pool_mixer(hx, hc, ab_w_in[j], ab_b_gate[j], ab_conv[j], ab_mnorm[j],
                                      ab_pool_w[j], ab_pool_scale[j], ab_w_out[j], not last)
        else:
            yx, yc = window_attention_mixer(hx, hc, c_w_in[j], c_sink[j], c_w_out[j], not last)
        x = x + gate[:, None] * rmsnorm(yx, g_post[l])
        if not last:
            ctx = ctx + gate_c * rmsnorm(yc, g_post[l])
    return x
```

```python
import math
import numpy as np
from contextlib import ExitStack
import concourse.bass as bass
import concourse.mybir as mybir
from concourse.alu_op_type import AluOpType as ALU
from concourse.bass_utils import run_bass_kernel_spmd

F32 = mybir.dt.float32
BF16 = mybir.dt.bfloat16
AF = mybir.ActivationFunctionType
AX = mybir.AxisListType

D = 1024
NCTX = 256
NX = 4096
NT = NCTX + NX
NTILE = NT // 128
AB_IN = 7184
GATE_OFF = 7168
C_IN = 2560
EPS = 1e-6
LN16 = math.log(1.0 / 16.0)


class Op:
    __slots__ = ("eng", "fn", "deps", "flag", "cnt", "stream")


class Prog:
    ENG = ("pe", "act", "dve", "pool", "sp")

    def __init__(self):
        self.nc = bass.Bass("TRN2", target_bir_lowering=False)
        self.es = ExitStack()
        self.ops = {e: [] for e in self.ENG}
        self.lastw = {}
        self.readers = {}
        self.streams = {}
        self.stage_stack = []
        self.stage_id = 0
        self.slot_of = {}
        self.nslot_stage = {}

    def sb(self, name, shape, dt):
        es = self.stage_stack[-1] if self.stage_stack else self.es
        self.uid = getattr(self, "uid", 0) + 1
        return es.enter_context(self.nc.sbuf_tensor(f"{name}_u{self.uid}", list(shape), dt))

    def ps(self, name, shape, dt):
        es = self.stage_stack[-1] if self.stage_stack else self.es
        self.uid = getattr(self, "uid", 0) + 1
        return es.enter_context(self.nc.psum_tensor(f"{name}_u{self.uid}", list(shape), dt))

    def dram(self, name, shape, dt, kind="Internal"):
        return self.nc.dram_tensor(name, list(shape), dt, kind=kind).ap()

    def begin_stage(self):
        self.stage_stack.append(ExitStack())

    def end_stage(self):
        self.barrier()
        self.split()
        self.stage_stack.pop().close()

    def op(self, eng, fn, r=(), w=(), dma=None):
        if dma is not None:
            sk = (self.stage_id, dma)
            if sk not in self.slot_of:
                n = self.nslot_stage.get(self.stage_id, 0)
                self.slot_of[sk] = n
                self.nslot_stage[self.stage_id] = n + 1
            dma = ("slot", self.slot_of[sk])
        o = Op()
        o.eng, o.fn, o.flag, o.stream, o.cnt = eng, fn, False, dma, 0
        deps = []
        for k in r:
            d = self.lastw.get(k)
            if d is not None:
                deps.append(d)
        for k in w:
            d = self.lastw.get(k)
            if d is not None:
                deps.append(d)
            deps.extend(self.readers.get(k, ()))
        ded = []
        seen = set()
        for d in deps:
            if id(d) in seen:
                continue
            seen.add(id(d))
            if d.stream is None and dma is None and d.eng == "pe" and eng == "pe":
                continue
            ded.append(d)
        o.deps = ded
        for d in ded:
            d.flag = True
        for k in r:
            lst = self.readers.setdefault(k, [])
            if dma is None:
                lst[:] = [x for x in lst if not (x.stream is None and x.eng == eng)]
            lst.append(o)
        for k in w:
            self.lastw[k] = o
            self.readers[k] = []
        self.ops[eng].append(o)
        if dma is not None:
            self.streams.setdefault(dma, []).append(o)
        return o

    def dma(self, eng, out, in_, r=(), w=(), stream=None):
        if stream is None:
            ks = [k for k in list(w) + list(r) if not (isinstance(k, str) and k.startswith("d:"))]
            stream = ("st", ks[0])
        return self.op(eng, lambda e: e.dma_start(out=out, in_=in_), r=r, w=w, dma=stream)

    def split(self):
        for e in self.ENG:
            o = Op()
            o.eng, o.fn, o.flag, o.stream, o.cnt = e, "SPLIT", False, None, 0
            o.deps = []
            self.ops[e].append(o)

    def barrier(self):
        lasts = []
        for e in self.ENG:
            for o in reversed(self.ops[e]):
                if o.stream is None and o.fn is not None and o.fn != "SPLIT":
                    lasts.append(o)
                    break
        for s, lst in self.streams.items():
            lasts.append(lst[-1])
        for d in lasts:
            d.flag = True
        for e in self.ENG:
            o = Op()
            o.eng, o.fn, o.flag, o.stream, o.cnt = e, None, False, None, 0
            o.deps = list(lasts)
            self.ops[e].append(o)
        self.lastw = {}
        self.readers = {}
        self.stage_id += 1

    def build(self):
        nc = self.nc
        self.barrier()
        sem = {e: self.es.enter_context(nc.semaphore(f"s_{e}")) for e in self.ENG}
        ssem = {}
        for i, s in enumerate(self.streams):
            ssem[s] = self.es.enter_context(nc.semaphore(f"d_{i}"))
        for e in self.ENG:
            c = 0
            for o in self.ops[e]:
                if o.stream is None and o.fn is not None and o.fn != "SPLIT":
                    if o.flag:
                        c += 1
                    o.cnt = c
        for s, lst in self.streams.items():
            for i, o in enumerate(lst):
                o.cnt = 16 * (i + 1)
        self.maxcnt = max([0] + [o.cnt for e in self.ENG for o in self.ops[e]])
        self.ninstr = sum(len(v) for v in self.ops.values())

        segs = {e: [[]] for e in self.ENG}
        for e in self.ENG:
            for o in self.ops[e]:
                if o.fn == "SPLIT":
                    segs[e].append([])
                else:
                    segs[e][-1].append(o)
        nseg = len(segs["pe"])
        seen_all = {e: {} for e in self.ENG}

        def run(e, eng, si):
            seen = seen_all[e]
            for o in segs[e][si]:
                need = {}
                for d in o.deps:
                    key = ("s", d.stream) if d.stream is not None else ("e", d.eng)
                    if d.cnt > need.get(key, 0):
                        need[key] = d.cnt
                for key, v in need.items():
                    if seen.get(key, 0) < v:
                        sm = ssem[key[1]] if key[0] == "s" else sem[key[1]]
                        eng.wait_ge(sm, v)
                        seen[key] = v
                if o.fn is None:
                    continue
                ins = o.fn(eng)
                if o.stream is not None:
                    ins.then_inc(ssem[o.stream], 16)
                elif o.flag:
                    ins.then_inc(sem[e], 1)

        for si in range(nseg):
            if not any(segs[e][si] for e in self.ENG):
                continue
            with nc.Block() as block:
                @block.tensor
                def _(eng):
                    run("pe", eng, si)

                @block.scalar
                def _(eng):
                    run("act", eng, si)

                @block.vector
                def _(eng):
                    run("dve", eng, si)

                @block.gpsimd
                def _(eng):
                    run("pool", eng, si)

                @block.sync
                def _(eng):
                    run("sp", eng, si)
        self.es.close()
        return nc

    def act(self, out, in_, func, r, w, bias=None, scale=None, accum=None):
        kw = {}
        if bias is not None:
            kw["bias"] = bias
        if scale is not None:
            kw["scale"] = scale
        if accum is not None:
            kw["accum_out"] = accum
        return self.op("act", lambda e: e.activation(out=out, in_=in_, func=func, **kw), r=r, w=w)

    def tt(self, eng, out, in0, in1, op, r, w):
        return self.op(eng, lambda e: e.tensor_tensor(out=out, in0=in0, in1=in1, op=op), r=r, w=w)

    def ts(self, eng, out, in0, s1, op0, r, w, s2=None, op1=None):
        if op1 is None:
            return self.op(eng, lambda e: e.tensor_scalar(out=out, in0=in0, scalar1=s1, scalar2=None, op0=op0), r=r, w=w)
        return self.op(eng, lambda e: e.tensor_scalar(out=out, in0=in0, scalar1=s1, scalar2=s2, op0=op0, op1=op1), r=r, w=w)

    def stt(self, out, in0, scalar, in1, op0, op1, r, w):
        return self.op("dve", lambda e: e.scalar_tensor_tensor(out=out, in0=in0, scalar=scalar, in1=in1, op0=op0, op1=op1), r=r, w=w)

    def cp(self, eng, out, in_, r, w):
        if eng == "act":
            return self.act(out, in_, AF.Copy, r, w)
        return self.op(eng, lambda e: e.tensor_copy(out=out, in_=in_), r=r, w=w)

    def mm(self, out, lhsT, rhs, start, stop, r, w):
        return self.op("pe", lambda e: e.matmul(out=out, lhsT=lhsT, rhs=rhs, start=start, stop=stop), r=r, w=w)

    def tr(self, out, in_, ident, r, w):
        return self.op("pe", lambda e: e.transpose(out=out, in_=in_, identity=ident), r=r, w=w)

    def memset(self, eng, ap, val, w):
        return self.op(eng, lambda e: e.memset(ap, val), r=(), w=w)

    def recip(self, out, in_, r, w):
        return self.op("dve", lambda e: e.reciprocal(out=out, in_=in_), r=r, w=w)


class Rot:
    def __init__(self, P, name, shape, dt, n, psum=False):
        self.t = [(P.ps if psum else P.sb)(f"{name}{i}", shape, dt) for i in range(n)]
        self.k = [f"{name}{i}" for i in range(n)]
        self.i = -1

    def next(self):
        self.i = (self.i + 1) % len(self.t)
        return self.t[self.i], self.k[self.i]


class Ctx:
    pass


def declare_io(P, layers, debug, single=False):
    g = Ctx()
    g.P = P
    g.layers = layers
    g.debug = debug
    g.single = single
    g.L = (lambda l: 0) if single else (lambda l: l)
    g.J = (lambda j: 0) if single else (lambda j: j)
    nl = 1 if single else 4
    nj = 1 if single else 2
    has_even = any(l % 2 == 0 for l in layers) or not single
    has_odd = any(l % 2 == 1 for l in layers) or not single
    I = lambda n, s: P.dram(n, s, F32, kind="ExternalInput")
    g.xc = I("xc", [NT, D])
    g.cs = I("cs", [128, 16])
    g.w_mod = I("w_mod", [nl, D, 3 * D])
    g.b_mod = I("b_mod", [nl, 3 * D])
    g.g_pre = I("g_pre", [nl, D])
    g.g_post = I("g_post", [nl, D])
    if has_even:
        g.ab_w_in = I("ab_w_in", [nj, D, AB_IN])
        g.ab_b_gate = I("ab_b_gate", [nj, 16])
        g.ab_conv = I("ab_conv", [nj, 128, 48])
        g.ab_mnorm = I("ab_mnorm", [nj, D])
        g.ab_pool_w = I("ab_pool_w", [nj, 4, 256, 256])
        g.ab_pool_scale = I("ab_pool_scale", [nj, 128, 8])
        g.ab_w_out = I("ab_w_out", [nj, 2 * D, D])
        g.k_invcnt = I("k_invcnt", [8, NX + 16])
    if has_odd:
        g.c_w_in = I("c_w_in", [nj, D, C_IN])
        g.c_sink = I("c_sink", [nj, 16])
        g.c_w_out = I("c_w_out", [nj, D, D])
        g.k_rope = I("k_rope", [NT, 64])
    g.k_ident = I("k_ident", [128, 128])
    g.k_trif = I("k_trif", [128, 128])
    g.k_trib = I("k_trib", [128, 128])
    if (not single) or 3 in layers:
        g.out = P.dram("out", [NX, D], F32, kind="ExternalOutput")
    dbg = set(debug) if debug else set()
    S = lambda n, s, dt: P.dram(n, s, dt, kind=("ExternalOutput" if n in dbg else "Internal"))
    g.xcs = [S("xc_a", [NT, D], F32), S("xc_b", [NT, D], F32)]
    g.modv = S("modv", [4, 2, 3, D], F32)
    if has_even:
        g.ut = S("ut", [4096, NT], BF16)
        g.utok = S("utok", [NT, 3072], BF16)
        g.gt = S("gt", [NT, 16], F32)
        g.hf = S("hf", [NT, D], F32)
        g.ypt = S("ypt", [D, NT], BF16)
    if has_odd:
        g.qt = S("qt", [NTILE, 128, 8, 128], BF16)
        g.zt = S("zt", [NT, D], BF16)
    return g


def load_consts(g):
    P = g.P
    g.identf = P.sb("identf", [128, 128], F32)
    g.ident = P.sb("ident", [128, 128], BF16)
    g.trif = P.sb("trif", [128, 128], F32)
    g.trib = P.sb("trib", [128, 128], F32)
    g.trifb = P.sb("trifb", [128, 128], BF16)
    g.tribb = P.sb("tribb", [128, 128], BF16)
    g.onesf = P.sb("onesf", [128, 128], F32)
    g.cc = P.sb("cc", [128, 8], F32)
    P.dma("sp", g.identf[:], g.k_ident, w=["identf"])
    P.dma("sp", g.trif[:], g.k_trif, w=["trif"])
    P.dma("sp", g.trib[:], g.k_trib, w=["trib"])
    P.cp("dve", g.ident[:], g.identf[:], r=["identf"], w=["ident"])
    P.cp("dve", g.trifb[:], g.trif[:], r=["trif"], w=["trifb"])
    P.cp("dve", g.tribb[:], g.trib[:], r=["trib"], w=["tribb"])
    P.memset("pool", g.onesf[:], 1.0, w=["onesf"])
    P.memset("pool", g.cc[:, 0:1], 0.0, w=["cc"])
    P.memset("pool", g.cc[:, 1:2], 1.0, w=["cc"])
    P.memset("pool", g.cc[:, 2:3], EPS, w=["cc"])
    P.memset("pool", g.cc[:, 3:4], LN16, w=["cc"])
    P.barrier()


def stage_mod(g, l):
    P = g.P
    P.begin_stage()
    wm = P.sb("wm", [128, 8, 3 * D], F32)
    cs = P.sb("cs_sb", [128, 16], F32)
    e1 = P.sb("m_e1", [128, 16], F32)
    sc = P.sb("m_sc", [128, 16], F32)
    bm = P.sb("m_bm", [2, 3 * D], F32)
    gp = P.sb("m_gp", [2, 2 * D], F32)
    res = P.sb("m_res", [2, 3 * D], F32)
    o3 = P.sb("m_o3", [2, 3 * D], F32)
    for k in range(8):
        P.dma("sp", wm[:, k, :], g.w_mod[g.L(l), k * 128:(k + 1) * 128, :], w=[f"wm{k}"])
    P.dma("sp", cs[:], g.cs, w=["cs"])
    P.dma("sp", bm[:], g.b_mod[g.L(l), :].partition_broadcast(2), w=["bm"])
    P.dma("sp", gp[:, 0:D], g.g_pre[g.L(l), :].partition_broadcast(2), w=["gp"], stream="gp0")
    P.dma("sp", gp[:, D:2 * D], g.g_post[g.L(l), :].partition_broadcast(2), w=["gp"], stream="gp1")
    P.act(e1[:], cs[:], AF.Exp, r=["cs"], w=["e1"], scale=-1.0)
    P.ts("dve", e1[:], e1[:], 1.0, ALU.add, r=["e1"], w=["e1"])
    P.recip(e1[:], e1[:], r=["e1"], w=["e1"])
    P.tt("dve", sc[:], cs[:], e1[:], ALU.mult, r=["cs", "e1"], w=["sc"])
    pm = [P.ps(f"m_ps{i}", [2, 512], F32) for i in range(6)]
    for n in range(6):
        for k in range(8):
            P.mm(pm[n][:], sc[:, 2 * k:2 * k + 2], wm[:, k, n * 512:(n + 1) * 512], k == 0, k == 7,
                 r=["sc", f"wm{k}"], w=[f"mps{n}"])
        P.tt("dve", res[:, n * 512:(n + 1) * 512], pm[n][:], bm[:, n * 512:(n + 1) * 512], ALU.add,
             r=[f"mps{n}", "bm"], w=["res"])
    P.stt(o3[:, 0:D], res[:, D:2 * D], 1.0, gp[:, 0:D], ALU.add, ALU.mult, r=["res", "gp"], w=["o3"])
    P.cp("dve", o3[:, D:2 * D], res[:, 0:D], r=["res"], w=["o3"])
    P.tt("dve", o3[:, 2 * D:3 * D], res[:, 2 * D:3 * D], gp[:, D:2 * D], ALU.mult, r=["res", "gp"], w=["o3"])
    P.dma("sp", g.modv[l].rearrange("s a d -> s (a d)"), o3[:], r=["o3"], w=["d:modv"])
    P.end_stage()


def norm_mod_tile(g, pf, xt, xk, A, B, src, cc, hb, hbk, junk, stat):
    P = g.P
    P.act(junk[:], xt[:], AF.Square, r=[xk], w=[pf + "junk", pf + "ss"], accum=stat[:, 0:1])
    P.act(stat[:, 1:2], stat[:, 0:1], AF.Ln, r=[pf + "ss"], w=[pf + "ln"], bias=cc[:, 2:3], scale=1.0 / D)
    P.act(stat[:, 2:3], stat[:, 1:2], AF.Exp, r=[pf + "ln"], w=[pf + "rstd"], scale=-0.5)
    P.stt(junk[:], xt[:], stat[:, 2:3], A[:, src, :], ALU.mult, ALU.mult, r=[xk, pf + "rstd", "AB"], w=[pf + "junk"])
    P.tt("pool", hb[:], junk[:], B[:, src, :], ALU.add, r=[pf + "junk", "AB"], w=[hbk])


def load_AB(g, l, A, B):
    P = g.P
    for s in range(2):
        P.dma("sp", A[:, s, :], g.modv[l, s, 0, :].partition_broadcast(128), w=["AB"], stream=f"ab{s}0")
        P.dma("sp", B[:, s, :], g.modv[l, s, 1, :].partition_broadcast(128), w=["AB"], stream=f"ab{s}1")


def blocks():
    return [(0, 2)] + [(2 + 4 * i, 4) for i in range(8)]


def stage_even_A(g, l, xin):
    P = g.P
    j = l // 2
    P.begin_stage()
    w = P.sb("wA", [128, 8, AB_IN], BF16)
    for k in range(8):
        P.dma("pool", w[:, k, :], g.ab_w_in[g.J(j), k * 128:(k + 1) * 128, :], w=[f"wA{k}"])
    wkeys = [f"wA{k}" for k in range(8)]
    A = P.sb("A_A", [128, 2, D], F32)
    B = P.sb("A_B", [128, 2, D], F32)
    load_AB(g, l, A, B)
    xr = Rot(P, "A_x", [128, D], F32, 2)
    junk = P.sb("A_junk", [128, D], F32)
    stat = Rot(P, "A_stat", [128, 4], F32, 2)
    hbr = Rot(P, "A_hb", [128, D], BF16, 2)
    hT = P.sb("A_hT", [128, 8, 512], BF16)
    ptr = Rot(P, "A_ptr", [128, D], BF16, 1, psum=True)
    pu = Rot(P, "A_pu", [128, 512], F32, 6, psum=True)
    utr = Rot(P, "A_ut", [128, 3072], BF16, 2)
    gtr = Rot(P, "A_gt", [128, 16], F32, 2)
    uTr = Rot(P, "A_uT", [128, 512], BF16, 3)
    tog = 0
    for (t0, nt) in blocks():
        P.split()
        src = 1 if t0 == 0 else 0
        TB = nt * 128
        for ti in range(nt):
            tile = t0 + ti
            xt, xk = xr.next()
            st, sk = stat.next()
            hb, hbk = hbr.next()
            P.dma("sp", xt[:], xin[tile * 128:(tile + 1) * 128, :], r=["d:xin"], w=[xk])
            norm_mod_tile(g, sk, xt, xk, A, B, src, g.cc, hb, hbk, junk, st)
            pt, ptk = ptr.next()
            for k in range(8):
                P.tr(pt[:, k * 128:(k + 1) * 128], hb[:, k * 128:(k + 1) * 128], g.ident[:], r=[hbk, "ident"], w=[ptk])
            P.cp("act", hT[:, :, ti * 128:(ti + 1) * 128], pt[:].rearrange("p (k t) -> p k t", k=8), r=[ptk], w=[f"hT{ti}"])
            ut, utk = utr.next()
            for n in range(6):
                ps, psk = pu.next()
                c0 = 2048 + n * 512
                for k in range(8):
                    P.mm(ps[:], hT[:, k, ti * 128:(ti + 1) * 128], w[:, k, c0:c0 + 512], k == 0, k == 7,
                         r=[f"hT{ti}", wkeys[k]], w=[psk])
                eng = "act" if (tog % 2 == 0) else "dve"
                tog += 1
                P.cp(eng, ut[:, n * 512:(n + 1) * 512], ps[:], r=[psk], w=[utk])
            P.dma("pool", g.utok[tile * 128:(tile + 1) * 128, :], ut[:], r=[utk], w=["d:utok"])
            ps, psk = pu.next()
            gtt, gtk = gtr.next()
            for k in range(8):
                P.mm(ps[:, 0:16], hT[:, k, ti * 128:(ti + 1) * 128], w[:, k, GATE_OFF:GATE_OFF + 16], k == 0, k == 7,
                     r=[f"hT{ti}", wkeys[k]], w=[psk])
            P.cp("dve", gtt[:], ps[:, 0:16], r=[psk], w=[gtk])
            P.dma("pool", g.gt[tile * 128:(tile + 1) * 128, :], gtt[:], r=[gtk], w=["d:gt"])
        hkeys = [f"hT{ti}" for ti in range(nt)]
        for n in range(32):
            c0 = n * 128 if n < 16 else 5120 + (n - 16) * 128
            ps, psk = pu.next()
            for k in range(8):
                P.mm(ps[:, 0:TB], w[:, k, c0:c0 + 128], hT[:, k, 0:TB], k == 0, k == 7, r=hkeys + [wkeys[k]], w=[psk])
            uT, uTk = uTr.next()
            eng = "act" if (tog % 2 == 0) else "dve"
            tog += 1
            P.cp(eng, uT[:, 0:TB], ps[:, 0:TB], r=[psk], w=[uTk])
            P.dma("pool", g.ut[n * 128:(n + 1) * 128, t0 * 128:t0 * 128 + TB], uT[:, 0:TB], r=[uTk], w=["d:ut"])
    P.end_stage()


def stage_even_P(g, l):
    P = g.P
    j = l // 2
    P.begin_stage()
    pw = P.sb("P_pw", [128, 8, 256], BF16)
    P.dma("pool", pw[:], g.ab_pool_w[g.J(j)].rearrange("g (cj p) d -> p (g cj) d", p=128), w=["pw"])
    psc = P.sb("P_psc", [128, 8], F32)
    P.dma("sp", psc[:], g.ab_pool_scale[g.J(j)], w=["psc"])
    PT = P.sb("P_PT", [128, 8, NT], BF16)
    inv = P.sb("P_inv", [128, NX + 16], F32)
    raw = Rot(P, "P_raw", [128, NX + 16], BF16, 2)
    f1 = P.sb("P_f1", [128, NX + 16], F32)
    f2 = P.sb("P_f2", [128, NX + 16], F32)
    for gi in range(4):
        for (n, tok0, invrow) in ((NX, NCTX, gi), (NCTX, 0, 4 + gi)):
            P.dma("sp", inv[:, 0:n + 16], g.k_invcnt[invrow, 0:n + 16].partition_broadcast(128), w=["inv"])
            for cj in range(2):
                ct = 2 * gi + cj
                rw, rwk = raw.next()
                P.memset("pool", rw[:, 0:8], 0.0, w=[rwk])
                P.memset("pool", rw[:, 8 + n:16 + n], 0.0, w=[rwk])
                P.dma("sp", rw[:, 8:8 + n], g.ut[2048 + ct * 128:2048 + (ct + 1) * 128, tok0:tok0 + n], r=["d:ut"], w=[rwk])
                P.tt("dve", f1[:, 1:15 + n], rw[:, 0:14 + n], rw[:, 1:15 + n], ALU.add, r=[rwk], w=["f1"])
                res, resk = f1, "f1"
                if gi >= 1:
                    P.tt("pool", f2[:, 2:14 + n], f1[:, 1:13 + n], f1[:, 3:15 + n], ALU.add, r=["f1"], w=["f2"])
                    res, resk = f2, "f2"
                if gi >= 2:
                    P.tt("dve", f1[:, 4:12 + n], f2[:, 2:10 + n], f2[:, 6:14 + n], ALU.add, r=["f2"], w=["f1"])
                    res, resk = f1, "f1"
                if gi >= 3:
                    P.tt("pool", f2[:, 8:8 + n], f1[:, 4:4 + n], f1[:, 12:12 + n], ALU.add, r=["f1"], w=["f2"])
                    res, resk = f2, "f2"
                P.tt("dve", res[:, 8:8 + n], res[:, 8:8 + n], inv[:, 8:8 + n], ALU.mult, r=[resk, "inv"], w=[resk])
                P.tt("pool", PT[:, ct, tok0:tok0 + n], res[:, 8:8 + n], rw[:, 8:8 + n], ALU.subtract, r=[resk, rwk], w=[f"PT{ct}"])
    pps = Rot(P, "P_ps", [128, 512], F32, 2, psum=True)
    zr = Rot(P, "P_z", [128, 512], BF16, 2)
    er = Rot(P, "P_e", [128, 512], F32, 2)
    yr = Rot(P, "P_y", [128, 512], BF16, 2)
    for (t0, nt) in blocks():
        P.split()
        TB = nt * 128
        tok0 = t0 * 128
        for gi in range(4):
            for dt in range(2):
                idx = 2 * gi + dt
                ps, psk = pps.next()
                for cj in range(2):
                    P.mm(ps[:, 0:TB], pw[:, gi * 2 + cj, dt * 128:(dt + 1) * 128], PT[:, 2 * gi + cj, tok0:tok0 + TB], cj == 0, cj == 1,
                         r=["pw", f"PT{2 * gi + cj}"], w=[psk])
                z, zk = zr.next()
                e, ek = er.next()
                y, yk = yr.next()
                P.dma("sp", z[:, 0:TB], g.ut[3072 + idx * 128:3072 + (idx + 1) * 128, tok0:tok0 + TB], r=["d:ut"], w=[zk])
                P.act(e[:, 0:TB], z[:, 0:TB], AF.Exp, r=[zk], w=[ek], scale=-1.0)
                P.ts("pool", e[:, 0:TB], e[:, 0:TB], 1.0, ALU.add, r=[ek], w=[ek])
                P.recip(e[:, 0:TB], e[:, 0:TB], r=[ek], w=[ek])
                P.tt("pool", e[:, 0:TB], e[:, 0:TB], z[:, 0:TB], ALU.mult, r=[ek, zk], w=[ek])
                P.stt(y[:, 0:TB], ps[:, 0:TB], psc[:, idx:idx + 1], e[:, 0:TB], ALU.mult, ALU.mult, r=[psk, "psc", ek], w=[yk])
                P.dma("pool", g.ypt[idx * 128:(idx + 1) * 128, tok0:tok0 + TB], y[:, 0:TB], r=[yk], w=["d:ypt"])
    P.end_stage()


def stage_even_S(g, l, xin, xout, last):
    P = g.P
    j = l // 2
    P.begin_stage()
    wo = P.sb("S_wo", [128, 16, D], BF16)
    for k in range(16):
        P.dma("pool", wo[:, k, :], g.ab_w_out[g.J(j), k * 128:(k + 1) * 128, :], w=[f"wo{k}"])
    cw = P.sb("S_cw", [128, 48], F32)
    P.dma("sp", cw[:], g.ab_conv[g.J(j)], w=["cw"])
    bg = P.sb("S_bg", [128, 16], F32)
    P.dma("sp", bg[:], g.ab_b_gate[g.J(j), :].partition_broadcast(128), w=["bg"])
    mn = P.sb("S_mn", [128, D], F32)
    P.dma("sp", mn[:], g.ab_mnorm[g.J(j), :].partition_broadcast(128), w=["mn"])
    GG = P.sb("S_GG", [128, 2, D], F32)
    for s in range(2):
        P.dma("sp", GG[:, s, :], g.modv[l, s, 2, :].partition_broadcast(128), w=["GG"], stream=f"gg{s}")
    Wb = [[P.sb(f"S_Wb{a}{t}", [128, 8, 128], F32) for t in range(3)] for a in range(2)]
    cwv = cw[:].rearrange("p (c t) -> p c t", t=3)
    for a in range(2):
        for t in range(3):
            P.cp("dve", Wb[a][t][:], cwv[:, a * 8:(a + 1) * 8, t:t + 1].to_broadcast([128, 8, 128]), r=["cw"], w=[f"Wb{a}{t}"])
    C = P.sb("S_C", [128, 4, 2, 257], F32)
    Cb = P.sb("S_Cb", [128, 4, 2, 257], BF16)
    raws = [Rot(P, "S_qr", [128, 8, 130], BF16, 2), Rot(P, "S_kr", [128, 8, 130], BF16, 2)]
    cvt = [P.sb(f"S_cvt{i}", [128, 8, 128], F32) for i in range(3)]
    qkc = [Rot(P, "S_qc", [128, 8, 128], BF16, 2), Rot(P, "S_kc", [128, 8, 128], BF16, 2)]
    ktokr = Rot(P, "S_ktok", [128, D], BF16, 2)
    vtokr = Rot(P, "S_vtok", [128, 3072], BF16, 2)
    gtr = Rot(P, "S_gt", [128, 16], F32, 2)
    gw = Rot(P, "S_gw", [128, 48], F32, 2)
    v1r = Rot(P, "S_v1", [128, 4, 257], BF16, 2)
    v2r = Rot(P, "S_v2", [128, 4, 257], BF16, 2)
    smr = Rot(P, "S_sm", [128, 128], BF16, 2)
    dnr = Rot(P, "S_dn", [128, 8], F32, 2)
    hhr = Rot(P, "S_hh", [128, D], F32, 2)
    hfr = Rot(P, "S_hf", [128, D], F32, 2)
    xr = Rot(P, "S_x", [128, D], F32, 2)
    ypr = Rot(P, "S_yp", [128, 8, 128], BF16, 2)
    t1 = P.sb("S_t1", [128, D], F32)
    t2 = P.sb("S_t2", [128, D], F32)
    t3 = P.sb("S_t3", [128, D], F32)
    st4 = Rot(P, "S_st4", [128, 16], F32, 2)
    ymr = Rot(P, "S_ym", [128, D], BF16, 2)
    ymTr = Rot(P, "S_ymT", [128, 8, 128], BF16, 2)
    xor_ = Rot(P, "S_xo", [128, D], F32, 2)
    p_tr = Rot(P, "S_ptr", [128, D], BF16, 1, psum=True)
    p_g = Rot(P, "S_pg", [128, 8], F32, 1, psum=True)
    p_s = Rot(P, "S_pst", [128, 128], F32, 1, psum=True)
    p_in = Rot(P, "S_pin", [128, 257], F32, 1, psum=True)
    p_c = [Rot(P, "S_pc0", [128, 257], F32, 1, psum=True), Rot(P, "S_pc1", [128, 257], F32, 1, psum=True)]
    p_wo = [Rot(P, "S_pw0", [128, 512], F32, 1, psum=True), Rot(P, "S_pw1", [128, 512], F32, 1, psum=True)]
    utq = [g.ut[0:1024, :].rearrange("(j p) t -> p j t", p=128), g.ut[1024:2048, :].rearrange("(j p) t -> p j t", p=128)]
    yptv = g.ypt.rearrange("(j p) t -> p j t", p=128)
    wokeys = [f"wo{k}" for k in range(16)]

    import os
    _dirs = tuple(int(c) for c in os.environ.get("KDIRS", "01"))
    _ntl = int(os.environ.get("KNT", "99"))
    _off = os.environ.get("KOFF", "")
    for dirn in _dirs:
        P.memset("pool", C[:], 0.0, w=["C"])
        P.memset("pool", Cb[:], 0.0, w=["Cb"])
        order = list(range(NTILE)) if dirn == 0 else [1, 0] + list(range(NTILE - 1, 1, -1))
        tri = g.trif if dirn == 0 else g.trib
        trik = "trif" if dirn == 0 else "trib"
        for tile in order[:_ntl]:
            P.split()
            isctx = tile < 2
            src = 1 if isctx else 0
            need_out = not (last and isctx)
            seq_lo = tile in (0, 2)
            seq_hi = tile in (1, NTILE - 1)
            tok = tile * 128
            c_lo = 1 if seq_lo else 0
            c_hi = 129 if seq_hi else 130
            conv = []
            for a in range(2):
                rw, rwk = raws[a].next()
                if seq_lo:
                    P.memset("pool", rw[:, :, 0:1], 0.0, w=[rwk])
                if seq_hi:
                    P.memset("pool", rw[:, :, 129:130], 0.0, w=[rwk])
                P.dma("sp", rw[:, :, c_lo:c_hi], utq[a][:, :, tok - 1 + c_lo:tok - 1 + c_hi], r=["d:ut"], w=[rwk])
                oc, ock = qkc[a].next()
                if a == 1 or need_out:
                    P.tt("pool", cvt[0][:], rw[:, :, 0:128], Wb[a][0][:], ALU.mult, r=[rwk, f"Wb{a}0"], w=["cvt0"])
                    P.tt("dve", cvt[1][:], rw[:, :, 1:129], Wb[a][1][:], ALU.mult, r=[rwk, f"Wb{a}1"], w=["cvt1"])
                    P.tt("pool", cvt[2][:], rw[:, :, 2:130], Wb[a][2][:], ALU.mult, r=[rwk, f"Wb{a}2"], w=["cvt2"])
                    P.tt("dve", cvt[0][:], cvt[0][:], cvt[1][:], ALU.add, r=["cvt0", "cvt1"], w=["cvt0"])
                    P.tt("pool", oc[:], cvt[0][:], cvt[2][:], ALU.add, r=["cvt0", "cvt2"], w=[ock])
                conv.append((oc, ock))
            (qc, qck), (kc, kck) = conv
            pt, ptk = p_tr.next()
            for ct in range(8):
                P.tr(pt[:, ct * 128:(ct + 1) * 128], kc[:, ct, :], g.ident[:], r=[kck, "ident"], w=[ptk])
            ktok, ktk = ktokr.next()
            P.cp("act", ktok[:], pt[:], r=[ptk], w=[ktk])
            vtok, vtk = vtokr.next()
            P.dma("sp", vtok[:], g.utok[tok:tok + 128, :], r=["d:utok"], w=[vtk])
            gtt, gtk = gtr.next()
            P.dma("sp", gtt[:], g.gt[tok:tok + 128, :], r=["d:gt"], w=[gtk])
            G, Gk = gw.next()
            gg = G[:, 0:16]
            l1, tmp, tmp2, ea, ea2, eb, edec, e1 = [G[:, 16 + 4 * i:20 + 4 * i] for i in range(8)]
            P.tt("pool", gg, gtt[:], bg[:], ALU.add, r=[gtk, "bg"], w=[Gk + "g"])
            li = G[:, 8 * dirn:8 * dirn + 4]
            fr = G[:, 8 * dirn + 4:8 * dirn + 8]
            P.act(e1, fr, AF.Exp, r=[Gk + "g"], w=[Gk + "e1"], scale=-1.0)
            P.act(l1, e1, AF.Ln, r=[Gk + "e1"], w=[Gk + "l1"], bias=g.cc[:, 1:2], scale=1.0)
            pg, pgk = p_g.next()
            P.mm(pg[:, 0:4], tri[:], l1, True, True, r=[trik, Gk + "l1"], w=[pgk])
            P.mm(pg[:, 4:8], g.onesf[:], l1, True, True, r=["onesf", Gk + "l1"], w=[pgk])
            P.tt("dve", tmp, li, pg[:, 0:4], ALU.add, r=[Gk + "g", pgk], w=[Gk + "tmp"])
            P.tt("dve", tmp2, tmp, pg[:, 4:8], ALU.subtract, r=[Gk + "tmp", pgk], w=[Gk + "tmp2"])
            P.act(ea, tmp, AF.Exp, r=[Gk + "tmp"], w=[Gk + "ea"], bias=g.cc[:, 3:4], scale=1.0)
            P.act(ea2, tmp2, AF.Exp, r=[Gk + "tmp2"], w=[Gk + "ea2"], bias=g.cc[:, 3:4], scale=1.0)
            P.act(eb, pg[:, 0:4], AF.Exp, r=[pgk], w=[Gk + "eb"], scale=-1.0)
            P.act(edec, pg[:, 4:8], AF.Exp, r=[pgk], w=[Gk + "edec"], scale=-1.0)
            v1, v1k = v1r.next()
            v2, v2k = v2r.next()
            for h in range(4):
                if need_out:
                    P.ts("dve", v1[:, h, 0:256], vtok[:, h * 256:(h + 1) * 256], ea[:, h:h + 1], ALU.mult, r=[vtk, Gk + "ea"], w=[v1k])
                P.ts("pool", v2[:, h, 0:256], vtok[:, h * 256:(h + 1) * 256], ea2[:, h:h + 1], ALU.mult, r=[vtk, Gk + "ea2"], w=[v2k])
            if need_out:
                P.cp("dve", v1[:, :, 256], ea, r=[Gk + "ea"], w=[v1k])
            P.cp("pool", v2[:, :, 256], ea2, r=[Gk + "ea2"], w=[v2k])
            hh, hhk = hhr.next()
            for h in range(4):
                if need_out:
                    ps_, psk_ = p_s.next()
                    for jj in range(2):
                        P.mm(ps_[:], kc[:, 2 * h + jj, :], qc[:, 2 * h + jj, :], jj == 0, jj == 1, r=[kck, qck], w=[psk_])
                    sm, smk = smr.next()
                    P.tt("dve", sm[:], ps_[:], tri[:], ALU.mult, r=[psk_, trik], w=[smk])
                    pi, pik = p_in.next()
                    for jj in range(2):
                        P.mm(pi[:], qc[:, 2 * h + jj, :], Cb[:, h, jj, :], jj == 0, False, r=[qck, "Cb"], w=[pik])
                    P.mm(pi[:], sm[:], v1[:, h, :], False, True, r=[smk, v1k], w=[pik])
                    dn, dnk = dnr.next()
                    P.ts("dve", dn[:, 0:1], pi[:, 256:257], eb[:, h:h + 1], ALU.mult, r=[pik, Gk + "eb"], w=[dnk])
                    P.stt(dn[:, 1:2], dn[:, 0:1], -1.0, dn[:, 0:1], ALU.mult, ALU.max, r=[dnk], w=[dnk])
                    P.ts("dve", dn[:, 2:3], dn[:, 1:2], 1.0, ALU.max, r=[dnk], w=[dnk])
                    P.recip(dn[:, 3:4], dn[:, 2:3], r=[dnk], w=[dnk])
                    P.tt("dve", dn[:, 4:5], dn[:, 3:4], eb[:, h:h + 1], ALU.mult, r=[dnk, Gk + "eb"], w=[dnk])
                    P.ts("dve", hh[:, h * 256:(h + 1) * 256], pi[:, 0:256], dn[:, 4:5], ALU.mult, r=[pik, dnk], w=[hhk])
                for jj in range(2):
                    pc, pck = p_c[jj].next()
                    P.mm(pc[:], ktok[:, h * 256 + jj * 128:h * 256 + (jj + 1) * 128], v2[:, h, :], True, True, r=[ktk, v2k], w=[pck])
                    P.stt(C[:, h, jj, :], C[:, h, jj, :], edec[:, h:h + 1], pc[:], ALU.mult, ALU.add, r=["C", Gk + "edec", pck], w=["C"])
                    P.cp("pool", Cb[:, h, jj, :], C[:, h, jj, :], r=["C"], w=["Cb"])
            if not need_out:
                continue
            if dirn == 0:
                P.dma("pool", g.hf[tok:tok + 128, :], hh[:], r=[hhk], w=["d:hf"])
                continue
            hf, hfk = hfr.next()
            P.dma("sp", hf[:], g.hf[tok:tok + 128, :], r=["d:hf"], w=[hfk])
            xt, xk = xr.next()
            P.dma("sp", xt[:], xin[tok:tok + 128, :], r=["d:xin"], w=[xk])
            yp, ypk = ypr.next()
            P.dma("sp", yp[:], yptv[:, :, tok:tok + 128], r=["d:ypt"], w=[ypk])
            P.tt("pool", hh[:], hh[:], hf[:], ALU.add, r=[hhk, hfk], w=[hhk])
            P.act(t1[:], vtok[:, 1024:2048], AF.Exp, r=[vtk], w=["t1"], scale=-1.0)
            P.ts("pool", t1[:], t1[:], 1.0, ALU.add, r=["t1"], w=["t1"])
            P.recip(t1[:], t1[:], r=["t1"], w=["t1"])
            P.tt("pool", hh[:], hh[:], t1[:], ALU.mult, r=[hhk, "t1"], w=[hhk])
            s4, s4k = st4.next()
            for h in range(4):
                P.act(t1[:, h * 256:(h + 1) * 256], hh[:, h * 256:(h + 1) * 256], AF.Square, r=[hhk], w=["t1", s4k],
                      accum=s4[:, h:h + 1])
            P.act(s4[:, 4:8], s4[:, 0:4], AF.Ln, r=[s4k], w=[s4k], bias=g.cc[:, 2:3], scale=1.0 / 256.0)
            P.act(s4[:, 8:12], s4[:, 4:8], AF.Exp, r=[s4k], w=[s4k], scale=-0.5)
            P.act(t2[:], vtok[:, 2048:3072], AF.Exp, r=[vtk], w=["t2"], scale=-1.0)
            P.ts("pool", t2[:], t2[:], 1.0, ALU.add, r=["t2"], w=["t2"])
            P.recip(t2[:], t2[:], r=["t2"], w=["t2"])
            P.tt("pool", t2[:], t2[:], vtok[:, 2048:3072], ALU.mult, r=["t2", vtk], w=["t2"])
            P.tt("pool", t2[:], t2[:], mn[:], ALU.mult, r=["t2", "mn"], w=["t2"])
            ym, ymk = ymr.next()
            for h in range(4):
                P.stt(ym[:, h * 256:(h + 1) * 256], hh[:, h * 256:(h + 1) * 256], s4[:, 8 + h:9 + h], t2[:, h * 256:(h + 1) * 256],
                      ALU.mult, ALU.mult, r=[hhk, s4k, "t2"], w=[ymk])
            pt, ptk = p_tr.next()
            for ct in range(8):
                P.tr(pt[:, ct * 128:(ct + 1) * 128], ym[:, ct * 128:(ct + 1) * 128], g.ident[:], r=[ymk, "ident"], w=[ptk])
            ymT, ymTk = ymTr.next()
            P.cp("act", ymT[:], pt[:].rearrange("p (k t) -> p k t", k=8), r=[ptk], w=[ymTk])
            pws = []
            for nt_ in range(2):
                pw_, pwk_ = p_wo[nt_].next()
                for f in range(16):
                    lhs = ymT[:, f, :] if f < 8 else yp[:, f - 8, :]
                    lk = ymTk if f < 8 else ypk
                    P.mm(pw_[:], lhs, wo[:, f, nt_ * 512:(nt_ + 1) * 512], f == 0, f == 15, r=[lk, wokeys[f]], w=[pwk_])
                pws.append((pw_, pwk_))
            post_norm_residual(g, pws, xt, xk, GG, src, t3, st4, xor_, xout, tile, last)
    P.end_stage()


def post_norm_residual(g, pws, xt, xk, GG, src, t3, st4, xor_, xout, tile, last):
    P = g.P
    s5, s5k = st4.next()
    for nt_ in range(2):
        pw_, pwk_ = pws[nt_]
        P.act(t3[:, nt_ * 512:(nt_ + 1) * 512], pw_[:], AF.Square, r=[pwk_], w=["t3", s5k], accum=s5[:, nt_:nt_ + 1])
    P.tt("dve", s5[:, 2:3], s5[:, 0:1], s5[:, 1:2], ALU.add, r=[s5k], w=[s5k])
    P.act(s5[:, 3:4], s5[:, 2:3], AF.Ln, r=[s5k], w=[s5k], bias=g.cc[:, 2:3], scale=1.0 / D)
    P.act(s5[:, 4:5], s5[:, 3:4], AF.Exp, r=[s5k], w=[s5k], scale=-0.5)
    xo, xok = xor_.next()
    for nt_ in range(2):
        pw_, pwk_ = pws[nt_]
        P.stt(t3[:, nt_ * 512:(nt_ + 1) * 512], pw_[:], s5[:, 4:5], GG[:, src, nt_ * 512:(nt_ + 1) * 512], ALU.mult, ALU.mult,
              r=[pwk_, s5k, "GG"], w=["t3"])
    P.tt("pool", xo[:], t3[:], xt[:], ALU.add, r=["t3", xk], w=[xok])
    tok = tile * 128
    if last:
        P.dma("pool", g.out[tok - NCTX:tok - NCTX + 128, :], xo[:], r=[xok], w=["d:xout"])
    else:
        P.dma("pool", xout[tok:tok + 128, :], xo[:], r=[xok], w=["d:xout"])


def stage_odd(g, l, xin, xout, last):
    P = g.P
    j = l // 2
    P.begin_stage()
    kT = P.sb("O_kT", [128, 4, NT], BF16)
    va = P.sb("O_va", [128, NTILE, 4, 65], BF16)
    P.memset("pool", va[:, :, :, 64:65], 1.0, w=["va_ones"])
    P.begin_stage()
    w = P.sb("OA_w", [128, 8, C_IN], BF16)
    for k in range(8):
        P.dma("pool", w[:, k, :], g.c_w_in[g.J(j), k * 128:(k + 1) * 128, :], w=[f"wC{k}"])
    wkeys = [f"wC{k}" for k in range(8)]
    A = P.sb("OA_A", [128, 2, D], F32)
    B = P.sb("OA_B", [128, 2, D], F32)
    load_AB(g, l, A, B)
    xr = Rot(P, "OA_x", [128, D], F32, 2)
    junk = P.sb("OA_junk", [128, D], F32)
    stat = Rot(P, "OA_stat", [128, 4], F32, 2)
    hbr = Rot(P, "OA_hb", [128, D], BF16, 2)
    hTr = Rot(P, "OA_hT", [128, 8, 128], BF16, 2)
    rpr = Rot(P, "OA_rp", [128, 64], F32, 2)
    tmr = [P.sb(f"OA_tm{i}", [128, 8, 32], F32) for i in range(4)]
    qrr = Rot(P, "OA_qr", [128, 16, 64], BF16, 2)
    kdr = Rot(P, "OA_kd", [128, 4, 2, 64], BF16, 2)
    qTr = Rot(P, "OA_qT", [128, 8, 128], BF16, 2)
    zbr = Rot(P, "OA_zb", [128, D], BF16, 2)
    p_h = Rot(P, "OA_ph", [128, D], BF16, 1, psum=True)
    p_u = Rot(P, "OA_pu", [128, 512], F32, 3, psum=True)
    p_q = Rot(P, "OA_pq", [128, D], BF16, 1, psum=True)
    p_k = Rot(P, "OA_pk", [128, 512], BF16, 1, psum=True)
    for tile in range(NTILE):
        P.split()
        src = 1 if tile < 2 else 0
        tok = tile * 128
        xt, xk = xr.next()
        st, sk = stat.next()
        hb, hbk = hbr.next()
        P.dma("sp", xt[:], xin[tok:tok + 128, :], r=["d:xin"], w=[xk])
        rp, rpk = rpr.next()
        P.dma("sp", rp[:], g.k_rope[tok:tok + 128, :], w=[rpk])
        norm_mod_tile(g, sk, xt, xk, A, B, src, g.cc, hb, hbk, junk, st)
        pt, ptk = p_h.next()
        for k in range(8):
            P.tr(pt[:, k * 128:(k + 1) * 128], hb[:, k * 128:(k + 1) * 128], g.ident[:], r=[hbk, "ident"], w=[ptk])
        hT, hTk = hTr.next()
        P.cp("act", hT[:], pt[:].rearrange("p (k t) -> p k t", k=8), r=[ptk], w=[hTk])
        qr, qrk = qrr.next()
        kd, kdk = kdr.next()
        zb, zbk = zbr.next()
        for n in range(5):
            ps, psk = p_u.next()
            for k in range(8):
                P.mm(ps[:], hT[:, k, :], w[:, k, n * 512:(n + 1) * 512], k == 0, k == 7, r=[hTk, wkeys[k]], w=[psk])
            if n < 2 or n == 2:
                nh = 8 if n < 2 else 4
                pv = ps[:, 0:nh * 64].rearrange("p (h i two) -> p h i two", h=nh, two=2)
                x1 = pv[:, :, :, 0]
                x2 = pv[:, :, :, 1]
                cosb = rp[:, 0:32].unsqueeze(1).to_broadcast([128, nh, 32])
                sinb = rp[:, 32:64].unsqueeze(1).to_broadcast([128, nh, 32])
                tm = [t[:, 0:nh, :] for t in tmr]
                P.tt("dve", tm[0], x1, cosb, ALU.mult, r=[psk, rpk], w=["tm0"])
                P.tt("dve", tm[1], x2, sinb, ALU.mult, r=[psk, rpk], w=["tm1"])
                P.tt("dve", tm[2], x1, sinb, ALU.mult, r=[psk, rpk], w=["tm2"])
                P.tt("dve", tm[3], x2, cosb, ALU.mult, r=[psk, rpk], w=["tm3"])
                if n < 2:
                    P.tt("pool", qr[:, 8 * n:8 * n + 8, 0:32], tm[0], tm[1], ALU.subtract, r=["tm0", "tm1"], w=[qrk])
                    P.tt("pool", qr[:, 8 * n:8 * n + 8, 32:64], tm[2], tm[3], ALU.add, r=["tm2", "tm3"], w=[qrk])
                else:
                    for c in range(2):
                        P.tt("pool", kd[:, :, c, 0:32], tm[0], tm[1], ALU.subtract, r=["tm0", "tm1"], w=[kdk])
                        P.tt("pool", kd[:, :, c, 32:64], tm[2], tm[3], ALU.add, r=["tm2", "tm3"], w=[kdk])
                    P.cp("act", va[:, tile, :, 0:64], ps[:, 256:512].rearrange("p (g d) -> p g d", g=4), r=[psk], w=[f"va{tile}"])
            else:
                P.cp("act", zb[:, (n - 3) * 512:(n - 2) * 512], ps[:], r=[psk], w=[zbk])
        P.dma("pool", g.zt[tok:tok + 128, :], zb[:], r=[zbk], w=["d:zt"])
        pq, pqk = p_q.next()
        qrf = qr[:].rearrange("p h d -> p (h d)")
        for k in range(8):
            P.tr(pq[:, k * 128:(k + 1) * 128], qrf[:, k * 128:(k + 1) * 128], g.ident[:], r=[qrk, "ident"], w=[pqk])
        qT, qTk = qTr.next()
        P.cp("dve", qT[:], pq[:].rearrange("p (k t) -> p k t", k=8), r=[pqk], w=[qTk])
        P.dma("pool", g.qt[tile], qT[:], r=[qTk], w=["d:qt"])
        pk, pkk = p_k.next()
        kdf = kd[:].rearrange("p g c d -> p (g c d)")
        for gq in range(4):
            P.tr(pk[:, gq * 128:(gq + 1) * 128], kdf[:, gq * 128:(gq + 1) * 128], g.ident[:], r=[kdk, "ident"], w=[pkk])
        P.cp("act", kT[:, :, tok:tok + 128], pk[:].rearrange("p (g t) -> p g t", g=4), r=[pkk], w=[f"kT{tile}"])
    P.end_stage()
    P.begin_stage()
    wo = P.sb("OB_wo", [128, 8, D], BF16)
    for k in range(8):
        P.dma("pool", wo[:, k, :], g.c_w_out[g.J(j), k * 128:(k + 1) * 128, :], w=[f"woC{k}"])
    snk = P.sb("OB_snk", [128, 16], F32)
    nsnk = P.sb("OB_nsnk", [128, 16], F32)
    P.dma("sp", snk[:], g.c_sink[g.J(j), :].partition_broadcast(128), w=["snk"])
    P.ts("dve", nsnk[:], snk[:], -1.0, ALU.mult, r=["snk"], w=["nsnk"])
    GG = P.sb("OB_GG", [128, 2, D], F32)
    for s_ in range(2):
        P.dma("sp", GG[:, s_, :], g.modv[l, s_, 2, :].partition_broadcast(128), w=["GG"], stream=f"ogg{s_}")
    qTr = Rot(P, "OB_qT", [128, 8, 128], BF16, 2)
    zr = Rot(P, "OB_z", [128, D], BF16, 2)
    xr = Rot(P, "OB_x", [128, D], F32, 2)
    mxr = Rot(P, "OB_mx", [128, 8], F32, 3)
    prr = Rot(P, "OB_pr", [128, 5, 128], BF16, 2)
    ptsr = Rot(P, "OB_pts", [128, 5, 128], BF16, 2)
    orr = Rot(P, "OB_o", [128, D], F32, 2)
    t1 = P.sb("OB_t1", [128, D], F32)
    t3 = P.sb("OB_t3", [128, D], F32)
    ogr = Rot(P, "OB_og", [128, D], BF16, 2)
    ogTr = Rot(P, "OB_ogT", [128, 8, 128], BF16, 2)
    st4 = Rot(P, "OB_st", [128, 16], F32, 2)
    xor_ = Rot(P, "OB_xo", [128, D], F32, 2)
    p_sc = Rot(P, "OB_psc", [128, 2, 512], F32, 1, psum=True)
    p_pt = Rot(P, "OB_ppt", [128, 5, 128], BF16, 1, psum=True)
    p_oa = Rot(P, "OB_poa", [128, 65], F32, 2, psum=True)
    p_og = Rot(P, "OB_pog", [128, D], BF16, 1, psum=True)
    p_wo = [Rot(P, "OB_pw0", [128, 512], F32, 1, psum=True), Rot(P, "OB_pw1", [128, 512], F32, 1, psum=True)]
    wokeys = [f"woC{k}" for k in range(8)]
    qtiles = list(range(2, NTILE)) + ([] if last else [0, 1])
    tog = 0
    for tile in qtiles:
        P.split()
        isctx = tile < 2
        src = 1 if isctx else 0
        tok = tile * 128
        band = [] if isctx else [t for t in (tile - 1, tile, tile + 1) if 2 <= t < NTILE]
        nbt = len(band)
        nkb = 2 + nbt
        qT, qTk = qTr.next()
        P.dma("sp", qT[:], g.qt[tile], r=["d:qt"], w=[qTk])
        z, zk = zr.next()
        P.dma("sp", z[:], g.zt[tok:tok + 128, :], r=["d:zt"], w=[zk])
        xt, xk = xr.next()
        P.dma("sp", xt[:], xin[tok:tok + 128, :], r=["d:xin"], w=[xk])
        o, ok_ = orr.next()
        for h in range(16):
            g_ = h // 4
            pb = (h % 2) * 64
            qtl = h // 2
            sc, sck = p_sc.next()
            P.mm(sc[:, 0, 0:256], qT[pb:pb + 64, qtl, :], kT[pb:pb + 64, g_, 0:256], True, True, r=[qTk], w=[sck])
            if nbt:
                b0 = band[0] * 128
                P.mm(sc[:, 1, 0:nbt * 128], qT[pb:pb + 64, qtl, :], kT[pb:pb + 64, g_, b0:b0 + nbt * 128], True, True, r=[qTk], w=[sck])
            mx, mxk = mxr.next()
            P.op("dve", lambda e, mx=mx, sc=sc: e.tensor_reduce(out=mx[:, 0:1], in_=sc[:, 0, 0:256], axis=AX.X, op=ALU.max), r=[sck], w=[mxk])
            if nbt:
                P.op("dve", lambda e, mx=mx, sc=sc, nbt=nbt: e.tensor_reduce(out=mx[:, 1:2], in_=sc[:, 1, 0:nbt * 128], axis=AX.X, op=ALU.max), r=[sck], w=[mxk])
                P.tt("dve", mx[:, 2:3], mx[:, 0:1], mx[:, 1:2], ALU.max, r=[mxk], w=[mxk])
                msrc = mx[:, 2:3]
            else:
                msrc = mx[:, 0:1]
            P.ts("dve", mx[:, 3:4], msrc, -0.125, ALU.mult, r=[mxk, "nsnk"], w=[mxk], s2=nsnk[:, h:h + 1], op1=ALU.min)
            pr, prk = prr.next()
            P.act(pr[:, 0:2, :], sc[:, 0, 0:256].rearrange("p (b s) -> p b s", b=2), AF.Exp, r=[sck, mxk], w=[prk], bias=mx[:, 3:4], scale=0.125)
            if nbt:
                P.act(pr[:, 2:2 + nbt, :], sc[:, 1, 0:nbt * 128].rearrange("p (b s) -> p b s", b=nbt), AF.Exp, r=[sck, mxk], w=[prk],
                      bias=mx[:, 3:4], scale=0.125)
            P.act(mx[:, 4:5], snk[:, h:h + 1], AF.Exp, r=["snk", mxk], w=[mxk], bias=mx[:, 3:4], scale=1.0)
            for bi, t in enumerate(band):
                if t == tile - 1:
                    P.tt("pool", pr[:, 2 + bi, :], pr[:, 2 + bi, :], g.trifb[:], ALU.mult, r=[prk, "trifb"], w=[prk])
                elif t == tile + 1:
                    P.tt("pool", pr[:, 2 + bi, :], pr[:, 2 + bi, :], g.tribb[:], ALU.mult, r=[prk, "tribb"], w=[prk])
            ptp, ptpk = p_pt.next()
            for b in range(nkb):
                P.tr(ptp[:, b, :], pr[:, b, :], g.ident[:], r=[prk, "ident"], w=[ptpk])
            pts, ptsk = ptsr.next()
            eng = "act" if tog % 2 == 0 else "dve"
            tog += 1
            P.cp(eng, pts[:, 0:nkb, :], ptp[:, 0:nkb, :], r=[ptpk], w=[ptsk])
            oa, oak = p_oa.next()
            for b, kt in enumerate([0, 1] + band):
                P.mm(oa[:], pts[:, b, :], va[:, kt, g_, :], b == 0, b == nkb - 1, r=[ptsk], w=[oak])
            P.tt("dve", mx[:, 5:6], oa[:, 64:65], mx[:, 4:5], ALU.add, r=[oak, mxk], w=[mxk])
            P.recip(mx[:, 6:7], mx[:, 5:6], r=[mxk], w=[mxk])
            P.ts("dve", o[:, h * 64:(h + 1) * 64], oa[:, 0:64], mx[:, 6:7], ALU.mult, r=[oak, mxk], w=[ok_])
        P.act(t1[:], z[:], AF.Exp, r=[zk], w=["t1"], scale=-1.0)
        P.ts("pool", t1[:], t1[:], 1.0, ALU.add, r=["t1"], w=["t1"])
        P.recip(t1[:], t1[:], r=["t1"], w=["t1"])
        P.tt("pool", t1[:], t1[:], z[:], ALU.mult, r=["t1", zk], w=["t1"])
        og, ogk = ogr.next()
        P.tt("pool", og[:], o[:], t1[:], ALU.mult, r=[ok_, "t1"], w=[ogk])
        pg_, pgk_ = p_og.next()
        for k in range(8):
            P.tr(pg_[:, k * 128:(k + 1) * 128], og[:, k * 128:(k + 1) * 128], g.ident[:], r=[ogk, "ident"], w=[pgk_])
        ogT, ogTk = ogTr.next()
        P.cp("act", ogT[:], pg_[:].rearrange("p (k t) -> p k t", k=8), r=[pgk_], w=[ogTk])
        pws = []
        for nt_ in range(2):
            pw_, pwk_ = p_wo[nt_].next()
            for f in range(8):
                P.mm(pw_[:], ogT[:, f, :], wo[:, f, nt_ * 512:(nt_ + 1) * 512], f == 0, f == 7, r=[ogTk, wokeys[f]], w=[pwk_])
            pws.append((pw_, pwk_))
        post_norm_residual(g, pws, xt, xk, GG, src, t3, st4, xor_, xout, tile, last)
    P.end_stage()
    P.end_stage()


def build_program(layers=(0, 1, 2, 3), debug=False, stages=None, single=False):
    P = Prog()
    g = declare_io(P, layers, debug, single)
    load_consts(g)
    xin = g.xc
    for l in layers:
        last = l == 3
        xout = g.xcs[l % 2]
        stage_mod(g, l)
        if l % 2 == 0:
            if stages is None or "A" in stages:
                stage_even_A(g, l, xin)
            if stages is None or "P" in stages:
                stage_even_P(g, l)
            if stages is None or "S" in stages:
                stage_even_S(g, l, xin, xout, last)
        else:
            stage_odd(g, l, xin, xout, last)
        xin = xout
    nc = P.build()
    return P, nc, g


def _const_tables():
    ident = np.eye(128, dtype=np.float32)
    s = np.arange(128)[:, None]
    t = np.arange(128)[None, :]
    trif = (s <= t).astype(np.float32)
    trib = (s >= t).astype(np.float32)
    inv = np.zeros((8, NX + 16), np.float32)
    for gi, wdw in enumerate((2, 4, 8, 16)):
        h = wdw // 2
        for row, n in ((gi, NX), (4 + gi, NCTX)):
            tt = np.arange(n)
            cnt = np.minimum(tt + h, n) - np.maximum(tt - h, 0)
            inv[row, 8:8 + n] = 1.0 / cnt
    rope = np.zeros((NT, 64), np.float32)
    rope[:NCTX, :32] = 1.0
    rows = NX // 64
    row = np.repeat(np.arange(rows), 64).astype(np.float32)
    col = np.tile(np.arange(64), rows).astype(np.float32)
    nf = 16
    invf = (10000.0 ** (-np.arange(nf, dtype=np.float32) / nf)).astype(np.float32)
    ang = np.concatenate([row[:, None] * invf, col[:, None] * invf], -1).astype(np.float32)
    rope[NCTX:, :32] = np.cos(ang)
    rope[NCTX:, 32:] = np.sin(ang)
    return dict(k_ident=ident, k_trif=trif, k_trib=trib, k_invcnt=inv, k_rope=rope)


def make_in_maps(inp, layer=None, xcs=None):
    f = lambda a: np.ascontiguousarray(np.asarray(a, dtype=np.float32))
    c, c_ctx = f(inp["c"]), f(inp["c_ctx"])
    ls = slice(None) if layer is None else slice(layer, layer + 1)
    js = slice(None) if layer is None else slice(layer // 2, layer // 2 + 1)
    even = layer is None or layer % 2 == 0
    odd = layer is None or layer % 2 == 1
    kt = _const_tables()
    shared = dict(w_mod=f(inp["w_mod"][ls]), b_mod=f(inp["b_mod"][ls]), g_pre=f(inp["g_pre"][ls]), g_post=f(inp["g_post"][ls]),
                  k_ident=kt["k_ident"], k_trif=kt["k_trif"], k_trib=kt["k_trib"])
    if even:
        shared.update(
            ab_w_in=f(inp["ab_w_in"][js]), ab_b_gate=f(inp["ab_b_gate"][js]),
            ab_conv=f(np.asarray(inp["ab_conv"]).reshape(2, 3, 16, 128).transpose(0, 3, 2, 1).reshape(2, 128, 48)[js]),
            ab_mnorm=f(inp["ab_mnorm"][js]), ab_pool_w=f(inp["ab_pool_w"][js]),
            ab_pool_scale=f(np.asarray(inp["ab_pool_scale"]).reshape(2, 8, 128).transpose(0, 2, 1)[js]),
            ab_w_out=f(inp["ab_w_out"][js]), k_invcnt=kt["k_invcnt"])
    if odd:
        shared.update(c_w_in=f(inp["c_w_in"][js]), c_sink=f(inp["c_sink"][js]), c_w_out=f(inp["c_w_out"][js]), k_rope=kt["k_rope"])
    maps = []
    for b in range(8):
        m = dict(shared)
        if xcs is None:
            m["xc"] = np.ascontiguousarray(np.concatenate([np.asarray(inp["ctx"][b], np.float32), np.asarray(inp["x"][b], np.float32)], axis=0))
        else:
            m["xc"] = np.ascontiguousarray(xcs[b])
        cs = np.zeros((128, 8, 2), np.float32)
        cs[:, :, 0] = c[b].reshape(8, 128).T
        cs[:, :, 1] = c_ctx.reshape(8, 128).T
        m["cs"] = np.ascontiguousarray(cs.reshape(128, 16))
        maps.append(m)
    return maps


def kernel(**inputs):
    xcs = None
    out = None
    for l in range(4):
        oname = "out" if l == 3 else ("xc_a" if l % 2 == 0 else "xc_b")
        P, nc, g = build_program(layers=(l,), debug=(oname,), single=True)
        maps = make_in_maps(inputs, layer=l, xcs=xcs)
        outs = []
        for b in range(8):
            res = run_bass_kernel_spmd(nc, [maps[b]], core_ids=[0])
            outs.append(np.asarray(res.results[0][oname], dtype=np.float32))
        if l < 3:
            xcs = outs
        else:
            out = np.stack(outs, axis=0)
    return out
```

```python
import math
import numpy as np
from contextlib import ExitStack
import concourse.bass as bass
import concourse.mybir as mybir
from concourse.alu_op_type import AluOpType as ALU
from concourse.bass_utils import run_bass_kernel_spmd

F32 = mybir.dt.float32
BF16 = mybir.dt.bfloat16
AF = mybir.ActivationFunctionType
AX = mybir.AxisListType

D = 1024
NCTX = 256
NX = 4096
NT = NCTX + NX
NTILE = NT // 128
AB_IN = 7184
GATE_OFF = 7168
C_IN = 2560
EPS = 1e-6
LN16 = math.log(1.0 / 16.0)


class Op:
    __slots__ = ("eng", "fn", "deps", "flag", "cnt", "stream")


class Prog:
    ENG = ("pe", "act", "dve", "pool", "sp")

    def __init__(self):
        self.nc = bass.Bass("TRN2", target_bir_lowering=False)
        self.es = ExitStack()
        self.ops = {e: [] for e in self.ENG}
        self.lastw = {}
        self.readers = {}
        self.streams = {}
        self.stage_stack = []
        self.stage_id = 0
        self.slot_of = {}
        self.nslot_stage = {}

    def sb(self, name, shape, dt):
        es = self.stage_stack[-1] if self.stage_stack else self.es
        self.uid = getattr(self, "uid", 0) + 1
        return es.enter_context(self.nc.sbuf_tensor(f"{name}_u{self.uid}", list(shape), dt))

    def ps(self, name, shape, dt):
        es = self.stage_stack[-1] if self.stage_stack else self.es
        self.uid = getattr(self, "uid", 0) + 1
        return es.enter_context(self.nc.psum_tensor(f"{name}_u{self.uid}", list(shape), dt))

    def dram(self, name, shape, dt, kind="Internal"):
        return self.nc.dram_tensor(name, list(shape), dt, kind=kind).ap()

    def begin_stage(self):
        self.stage_stack.append(ExitStack())

    def end_stage(self):
        self.barrier()
        self.split()
        self.stage_stack.pop().close()

    def op(self, eng, fn, r=(), w=(), dma=None):
        if dma is not None:
            sk = (self.stage_id, dma)
            if sk not in self.slot_of:
                n = self.nslot_stage.get(self.stage_id, 0)
                self.slot_of[sk] = n
                self.nslot_stage[self.stage_id] = n + 1
            dma = ("slot", self.slot_of[sk])
        o = Op()
        o.eng, o.fn, o.flag, o.stream, o.cnt = eng, fn, False, dma, 0
        deps = []
        for k in r:
            d = self.lastw.get(k)
            if d is not None:
                deps.append(d)
        for k in w:
            d = self.lastw.get(k)
            if d is not None:
                deps.append(d)
            deps.extend(self.readers.get(k, ()))
        ded = []
        seen = set()
        for d in deps:
            if id(d) in seen:
                continue
            seen.add(id(d))
            if d.stream is None and dma is None and d.eng == "pe" and eng == "pe":
                continue
            ded.append(d)
        o.deps = ded
        for d in ded:
            d.flag = True
        for k in r:
            lst = self.readers.setdefault(k, [])
            if dma is None:
                lst[:] = [x for x in lst if not (x.stream is None and x.eng == eng)]
            lst.append(o)
        for k in w:
            self.lastw[k] = o
            self.readers[k] = []
        self.ops[eng].append(o)
        if dma is not None:
            self.streams.setdefault(dma, []).append(o)
        return o

    def dma(self, eng, out, in_, r=(), w=(), stream=None):
        if stream is None:
            ks = [k for k in list(w) + list(r) if not (isinstance(k, str) and k.startswith("d:"))]
            stream = ("st", ks[0])
        return self.op(eng, lambda e: e.dma_start(out=out, in_=in_), r=r, w=w, dma=stream)

    def split(self):
        for e in self.ENG:
            o = Op()
            o.eng, o.fn, o.flag, o.stream, o.cnt = e, "SPLIT", False, None, 0
            o.deps = []
            self.ops[e].append(o)

    def barrier(self):
        lasts = []
        for e in self.ENG:
            for o in reversed(self.ops[e]):
                if o.stream is None and o.fn is not None and o.fn != "SPLIT":
                    lasts.append(o)
                    break
        for s, lst in self.streams.items():
            lasts.append(lst[-1])
        for d in lasts:
            d.flag = True
        for e in self.ENG:
            o = Op()
            o.eng, o.fn, o.flag, o.stream, o.cnt = e, None, False, None, 0
            o.deps = list(lasts)
            self.ops[e].append(o)
        self.lastw = {}
        self.readers = {}
        self.stage_id += 1

    def build(self):
        nc = self.nc
        self.barrier()
        sem = {e: self.es.enter_context(nc.semaphore(f"s_{e}")) for e in self.ENG}
        ssem = {}
        for i, s in enumerate(self.streams):
            ssem[s] = self.es.enter_context(nc.semaphore(f"d_{i}"))
        for e in self.ENG:
            c = 0
            for o in self.ops[e]:
                if o.stream is None and o.fn is not None and o.fn != "SPLIT":
                    if o.flag:
                        c += 1
                    o.cnt = c
        for s, lst in self.streams.items():
            for i, o in enumerate(lst):
                o.cnt = 16 * (i + 1)
        self.maxcnt = max([0] + [o.cnt for e in self.ENG for o in self.ops[e]])
        self.ninstr = sum(len(v) for v in self.ops.values())

        segs = {e: [[]] for e in self.ENG}
        for e in self.ENG:
            for o in self.ops[e]:
                if o.fn == "SPLIT":
                    segs[e].append([])
                else:
                    segs[e][-1].append(o)
        nseg = len(segs["pe"])
        seen_all = {e: {} for e in self.ENG}

        def run(e, eng, si):
            seen = seen_all[e]
            for o in segs[e][si]:
                need = {}
                for d in o.deps:
                    key = ("s", d.stream) if d.stream is not None else ("e", d.eng)
                    if d.cnt > need.get(key, 0):
                        need[key] = d.cnt
                for key, v in need.items():
                    if seen.get(key, 0) < v:
                        sm = ssem[key[1]] if key[0] == "s" else sem[key[1]]
                        eng.wait_ge(sm, v)
                        seen[key] = v
                if o.fn is None:
                    continue
                ins = o.fn(eng)
                if o.stream is not None:
                    ins.then_inc(ssem[o.stream], 16)
                elif o.flag:
                    ins.then_inc(sem[e], 1)

        for si in range(nseg):
            if not any(segs[e][si] for e in self.ENG):
                continue
            with nc.Block() as block:
                @block.tensor
                def _(eng):
                    run("pe", eng, si)

                @block.scalar
                def _(eng):
                    run("act", eng, si)

                @block.vector
                def _(eng):
                    run("dve", eng, si)

                @block.gpsimd
                def _(eng):
                    run("pool", eng, si)

                @block.sync
                def _(eng):
                    run("sp", eng, si)
        self.es.close()
        return nc

    def act(self, out, in_, func, r, w, bias=None, scale=None, accum=None):
        kw = {}
        if bias is not None:
            kw["bias"] = bias
        if scale is not None:
            kw["scale"] = scale
        if accum is not None:
            kw["accum_out"] = accum
        return self.op("act", lambda e: e.activation(out=out, in_=in_, func=func, **kw), r=r, w=w)

    def tt(self, eng, out, in0, in1, op, r, w):
        return self.op(eng, lambda e: e.tensor_tensor(out=out, in0=in0, in1=in1, op=op), r=r, w=w)

    def ts(self, eng, out, in0, s1, op0, r, w, s2=None, op1=None):
        if op1 is None:
            return self.op(eng, lambda e: e.tensor_scalar(out=out, in0=in0, scalar1=s1, scalar2=None, op0=op0), r=r, w=w)
        return self.op(eng, lambda e: e.tensor_scalar(out=out, in0=in0, scalar1=s1, scalar2=s2, op0=op0, op1=op1), r=r, w=w)

    def stt(self, out, in0, scalar, in1, op0, op1, r, w):
        return self.op("dve", lambda e: e.scalar_tensor_tensor(out=out, in0=in0, scalar=scalar, in1=in1, op0=op0, op1=op1), r=r, w=w)

    def cp(self, eng, out, in_, r, w):
        if eng == "act":
            return self.act(out, in_, AF.Copy, r, w)
        return self.op(eng, lambda e: e.tensor_copy(out=out, in_=in_), r=r, w=w)

    def mm(self, out, lhsT, rhs, start, stop, r, w):
        return self.op("pe", lambda e: e.matmul(out=out, lhsT=lhsT, rhs=rhs, start=start, stop=stop), r=r, w=w)

    def tr(self, out, in_, ident, r, w):
        return self.op("pe", lambda e: e.transpose(out=out, in_=in_, identity=ident), r=r, w=w)

    def memset(self, eng, ap, val, w):
        return self.op(eng, lambda e: e.memset(ap, val), r=(), w=w)

    def recip(self, out, in_, r, w):
        return self.op("dve", lambda e: e.reciprocal(out=out, in_=in_), r=r, w=w)


class Rot:
    def __init__(self, P, name, shape, dt, n, psum=False):
        self.t = [(P.ps if psum else P.sb)(f"{name}{i}", shape, dt) for i in range(n)]
        self.k = [f"{name}{i}" for i in range(n)]
        self.i = -1

    def next(self):
        self.i = (self.i + 1) % len(self.t)
        return self.t[self.i], self.k[self.i]


class Ctx:
    pass


def declare_io(P, layers, debug, single=False):
    g = Ctx()
    g.P = P
    g.layers = layers
    g.debug = debug
    g.single = single
    g.L = (lambda l: 0) if single else (lambda l: l)
    g.J = (lambda j: 0) if single else (lambda j: j)
    nl = 1 if single else 4
    nj = 1 if single else 2
    has_even = any(l % 2 == 0 for l in layers) or not single
    has_odd = any(l % 2 == 1 for l in layers) or not single
    I = lambda n, s: P.dram(n, s, F32, kind="ExternalInput")
    g.xc = I("xc", [NT, D])
    g.cs = I("cs", [128, 16])
    g.w_mod = I("w_mod", [nl, D, 3 * D])
    g.b_mod = I("b_mod", [nl, 3 * D])
    g.g_pre = I("g_pre", [nl, D])
    g.g_post = I("g_post", [nl, D])
    if has_even:
        g.ab_w_in = I("ab_w_in", [nj, D, AB_IN])
        g.ab_b_gate = I("ab_b_gate", [nj, 16])
        g.ab_conv = I("ab_conv", [nj, 128, 48])
        g.ab_mnorm = I("ab_mnorm", [nj, D])
        g.ab_pool_w = I("ab_pool_w", [nj, 4, 256, 256])
        g.ab_pool_scale = I("ab_pool_scale", [nj, 128, 8])
        g.ab_w_out = I("ab_w_out", [nj, 2 * D, D])
        g.k_invcnt = I("k_invcnt", [8, NX + 16])
    if has_odd:
        g.c_w_in = I("c_w_in", [nj, D, C_IN])
        g.c_sink = I("c_sink", [nj, 16])
        g.c_w_out = I("c_w_out", [nj, D, D])
        g.k_rope = I("k_rope", [NT, 64])
    g.k_ident = I("k_ident", [128, 128])
    g.k_trif = I("k_trif", [128, 128])
    g.k_trib = I("k_trib", [128, 128])
    if (not single) or 3 in layers:
        g.out = P.dram("out", [NX, D], F32, kind="ExternalOutput")
    dbg = set(debug) if debug else set()
    S = lambda n, s, dt: P.dram(n, s, dt, kind=("ExternalOutput" if n in dbg else "Internal"))
    g.xcs = [S("xc_a", [NT, D], F32), S("xc_b", [NT, D], F32)]
    g.modv = S("modv", [4, 2, 3, D], F32)
    if has_even:
        g.ut = S("ut", [4096, NT], BF16)
        g.utok = S("utok", [NT, 3072], BF16)
        g.gt = S("gt", [NT, 16], F32)
        g.hf = S("hf", [NT, D], F32)
        g.ypt = S("ypt", [D, NT], BF16)
    if has_odd:
        g.qt = S("qt", [NTILE, 128, 8, 128], BF16)
        g.zt = S("zt", [NT, D], BF16)
    return g


def load_consts(g):
    P = g.P
    g.identf = P.sb("identf", [128, 128], F32)
    g.ident = P.sb("ident", [128, 128], BF16)
    g.trif = P.sb("trif", [128, 128], F32)
    g.trib = P.sb("trib", [128, 128], F32)
    g.trifb = P.sb("trifb", [128, 128], BF16)
    g.tribb = P.sb("tribb", [128, 128], BF16)
    g.onesf = P.sb("onesf", [128, 128], F32)
    g.onesb = P.sb("onesb", [128, 128], BF16)
    g.cc = P.sb("cc", [128, 8], F32)
    P.dma("sp", g.identf[:], g.k_ident, w=["identf"])
    P.dma("sp", g.trif[:], g.k_trif, w=["trif"])
    P.dma("sp", g.trib[:], g.k_trib, w=["trib"])
    P.cp("dve", g.ident[:], g.identf[:], r=["identf"], w=["ident"])
    P.cp("dve", g.trifb[:], g.trif[:], r=["trif"], w=["trifb"])
    P.cp("dve", g.tribb[:], g.trib[:], r=["trib"], w=["tribb"])
    P.memset("pool", g.onesf[:], 1.0, w=["onesf"])
    P.memset("pool", g.onesb[:], 1.0, w=["onesb"])
    P.memset("pool", g.cc[:, 0:1], 0.0, w=["cc"])
    P.memset("pool", g.cc[:, 1:2], 1.0, w=["cc"])
    P.memset("pool", g.cc[:, 2:3], EPS, w=["cc"])
    P.memset("pool", g.cc[:, 3:4], LN16, w=["cc"])
    P.barrier()


def stage_mod(g, l):
    P = g.P
    P.begin_stage()
    wmr = Rot(P, "wm", [128, 3 * D], F32, 2)
    whr = Rot(P, "wh", [128, 3 * D], BF16, 2)
    wlr = Rot(P, "wl", [128, 3 * D], BF16, 2)
    cs = P.sb("cs_sb", [128, 16], F32)
    e1 = P.sb("m_e1", [128, 16], F32)
    sc = P.sb("m_sc", [128, 16], F32)
    sch = P.sb("m_sch", [128, 16], BF16)
    scl = P.sb("m_scl", [128, 16], BF16)
    bm = P.sb("m_bm", [2, 3 * D], F32)
    gp = P.sb("m_gp", [2, 2 * D], F32)
    res = P.sb("m_res", [2, 3 * D], F32)
    o3 = P.sb("m_o3", [2, 3 * D], F32)
    P.dma("sp", cs[:], g.cs, w=["cs"])
    P.dma("sp", bm[:], g.b_mod[g.L(l), :].partition_broadcast(2), w=["bm"])
    P.dma("sp", gp[:, 0:D], g.g_pre[g.L(l), :].partition_broadcast(2), w=["gp"], stream="gp0")
    P.dma("sp", gp[:, D:2 * D], g.g_post[g.L(l), :].partition_broadcast(2), w=["gp"], stream="gp1")
    P.act(e1[:], cs[:], AF.Exp, r=["cs"], w=["e1"], scale=-1.0)
    P.ts("dve", e1[:], e1[:], 1.0, ALU.add, r=["e1"], w=["e1"])
    P.recip(e1[:], e1[:], r=["e1"], w=["e1"])
    P.tt("dve", sc[:], cs[:], e1[:], ALU.mult, r=["cs", "e1"], w=["sc"])
    P.cp("dve", sch[:], sc[:], r=["sc"], w=["sch"])
    P.tt("dve", scl[:], sc[:], sch[:], ALU.subtract, r=["sc", "sch"], w=["scl"])
    pm = [P.ps(f"m_ps{i}", [2, 512], F32) for i in range(6)]
    for k in range(8):
        wm, wmk = wmr.next()
        wh, whk = whr.next()
        wl, wlk = wlr.next()
        P.dma("sp", wm[:], g.w_mod[g.L(l), k * 128:(k + 1) * 128, :], w=[wmk])
        P.cp("act", wh[:], wm[:], r=[wmk], w=[whk])
        P.tt("dve", wl[:], wm[:], wh[:], ALU.subtract, r=[wmk, whk], w=[wlk])
        for n in range(6):
            cs_ = slice(n * 512, (n + 1) * 512)
            P.mm(pm[n][:], sch[:, 2 * k:2 * k + 2], wh[:, cs_], k == 0, False, r=["sch", whk], w=[f"mps{n}"])
            P.mm(pm[n][:], scl[:, 2 * k:2 * k + 2], wh[:, cs_], False, False, r=["scl", whk], w=[f"mps{n}"])
            P.mm(pm[n][:], sch[:, 2 * k:2 * k + 2], wl[:, cs_], False, k == 7, r=["sch", wlk], w=[f"mps{n}"])
    for n in range(6):
        P.tt("dve", res[:, n * 512:(n + 1) * 512], pm[n][:], bm[:, n * 512:(n + 1) * 512], ALU.add,
             r=[f"mps{n}", "bm"], w=["res"])
    P.stt(o3[:, 0:D], res[:, D:2 * D], 1.0, gp[:, 0:D], ALU.add, ALU.mult, r=["res", "gp"], w=["o3"])
    P.cp("dve", o3[:, D:2 * D], res[:, 0:D], r=["res"], w=["o3"])
    P.tt("dve", o3[:, 2 * D:3 * D], res[:, 2 * D:3 * D], gp[:, D:2 * D], ALU.mult, r=["res", "gp"], w=["o3"])
    P.dma("sp", g.modv[l].rearrange("s a d -> s (a d)"), o3[:], r=["o3"], w=["d:modv"])
    P.end_stage()


def norm_mod_tile(g, pf, xt, xk, A, B, src, cc, hb, hbk, junk, stat):
    P = g.P
    P.act(junk[:], xt[:], AF.Square, r=[xk], w=[pf + "junk", pf + "ss"], accum=stat[:, 0:1])
    P.act(stat[:, 1:2], stat[:, 0:1], AF.Ln, r=[pf + "ss"], w=[pf + "ln"], bias=cc[:, 2:3], scale=1.0 / D)
    P.act(stat[:, 2:3], stat[:, 1:2], AF.Exp, r=[pf + "ln"], w=[pf + "rstd"], scale=-0.5)
    P.stt(junk[:], xt[:], stat[:, 2:3], A[:, src, :], ALU.mult, ALU.mult, r=[xk, pf + "rstd", "AB"], w=[pf + "junk"])
    P.tt("pool", hb[:], junk[:], B[:, src, :], ALU.add, r=[pf + "junk", "AB"], w=[hbk])


def load_AB(g, l, A, B):
    P = g.P
    for s in range(2):
        P.dma("sp", A[:, s, :], g.modv[l, s, 0, :].partition_broadcast(128), w=["AB"], stream=f"ab{s}0")
        P.dma("sp", B[:, s, :], g.modv[l, s, 1, :].partition_broadcast(128), w=["AB"], stream=f"ab{s}1")


def blocks():
    return [(0, 2)] + [(2 + 4 * i, 4) for i in range(8)]


def stage_even_A(g, l, xin):
    P = g.P
    j = l // 2
    P.begin_stage()
    w = P.sb("wA", [128, 8, AB_IN], BF16)
    for k in range(8):
        P.dma("pool", w[:, k, :], g.ab_w_in[g.J(j), k * 128:(k + 1) * 128, :], w=[f"wA{k}"])
    wkeys = [f"wA{k}" for k in range(8)]
    A = P.sb("A_A", [128, 2, D], F32)
    B = P.sb("A_B", [128, 2, D], F32)
    load_AB(g, l, A, B)
    xr = Rot(P, "A_x", [128, D], F32, 2)
    junk = P.sb("A_junk", [128, D], F32)
    stat = Rot(P, "A_stat", [128, 4], F32, 2)
    hbr = Rot(P, "A_hb", [128, D], BF16, 2)
    hT = P.sb("A_hT", [128, 8, 512], BF16)
    ptr = Rot(P, "A_ptr", [128, D], BF16, 1, psum=True)
    pu = Rot(P, "A_pu", [128, 512], F32, 6, psum=True)
    utr = Rot(P, "A_ut", [128, 3072], BF16, 2)
    gtr = Rot(P, "A_gt", [128, 16], F32, 2)
    uTr = Rot(P, "A_uT", [128, 512], BF16, 3)
    tog = 0
    for (t0, nt) in blocks():
        P.split()
        src = 1 if t0 == 0 else 0
        TB = nt * 128
        for ti in range(nt):
            tile = t0 + ti
            xt, xk = xr.next()
            st, sk = stat.next()
            hb, hbk = hbr.next()
            P.dma("sp", xt[:], xin[tile * 128:(tile + 1) * 128, :], r=["d:xin"], w=[xk])
            norm_mod_tile(g, sk, xt, xk, A, B, src, g.cc, hb, hbk, junk, st)
            pt, ptk = ptr.next()
            for k in range(8):
                P.tr(pt[:, k * 128:(k + 1) * 128], hb[:, k * 128:(k + 1) * 128], g.ident[:], r=[hbk, "ident"], w=[ptk])
            P.cp("act", hT[:, :, ti * 128:(ti + 1) * 128], pt[:].rearrange("p (k t) -> p k t", k=8), r=[ptk], w=[f"hT{ti}"])
            ut, utk = utr.next()
            for n in range(6):
                ps, psk = pu.next()
                c0 = 2048 + n * 512
                for k in range(8):
                    P.mm(ps[:], hT[:, k, ti * 128:(ti + 1) * 128], w[:, k, c0:c0 + 512], k == 0, k == 7,
                         r=[f"hT{ti}", wkeys[k]], w=[psk])
                eng = "act" if (tog % 2 == 0) else "dve"
                tog += 1
                P.cp(eng, ut[:, n * 512:(n + 1) * 512], ps[:], r=[psk], w=[utk])
            P.dma("pool", g.utok[tile * 128:(tile + 1) * 128, :], ut[:], r=[utk], w=["d:utok"])
            ps, psk = pu.next()
            gtt, gtk = gtr.next()
            for k in range(8):
                P.mm(ps[:, 0:16], hT[:, k, ti * 128:(ti + 1) * 128], w[:, k, GATE_OFF:GATE_OFF + 16], k == 0, k == 7,
                     r=[f"hT{ti}", wkeys[k]], w=[psk])
            P.cp("dve", gtt[:], ps[:, 0:16], r=[psk], w=[gtk])
            P.dma("pool", g.gt[tile * 128:(tile + 1) * 128, :], gtt[:], r=[gtk], w=["d:gt"])
        hkeys = [f"hT{ti}" for ti in range(nt)]
        for n in range(32):
            c0 = n * 128 if n < 16 else 5120 + (n - 16) * 128
            ps, psk = pu.next()
            for k in range(8):
                P.mm(ps[:, 0:TB], w[:, k, c0:c0 + 128], hT[:, k, 0:TB], k == 0, k == 7, r=hkeys + [wkeys[k]], w=[psk])
            uT, uTk = uTr.next()
            eng = "act" if (tog % 2 == 0) else "dve"
            tog += 1
            P.cp(eng, uT[:, 0:TB], ps[:, 0:TB], r=[psk], w=[uTk])
            P.dma("pool", g.ut[n * 128:(n + 1) * 128, t0 * 128:t0 * 128 + TB], uT[:, 0:TB], r=[uTk], w=["d:ut"])
    P.end_stage()


def stage_even_P(g, l):
    P = g.P
    j = l // 2
    P.begin_stage()
    pw = P.sb("P_pw", [128, 8, 256], BF16)
    P.dma("pool", pw[:], g.ab_pool_w[g.J(j)].rearrange("g (cj p) d -> p (g cj) d", p=128), w=["pw"])
    psc = P.sb("P_psc", [128, 8], F32)
    P.dma("sp", psc[:], g.ab_pool_scale[g.J(j)], w=["psc"])
    PT = P.sb("P_PT", [128, 8, NT], BF16)
    inv = P.sb("P_inv", [128, NX + 16], F32)
    raw = Rot(P, "P_raw", [128, NX + 16], BF16, 2)
    f1 = P.sb("P_f1", [128, NX + 16], F32)
    f2 = P.sb("P_f2", [128, NX + 16], F32)
    for gi in range(4):
        for (n, tok0, invrow) in ((NX, NCTX, gi), (NCTX, 0, 4 + gi)):
            P.dma("sp", inv[:, 0:n + 16], g.k_invcnt[invrow, 0:n + 16].partition_broadcast(128), w=["inv"])
            for cj in range(2):
                ct = 2 * gi + cj
                rw, rwk = raw.next()
                P.memset("pool", rw[:, 0:8], 0.0, w=[rwk])
                P.memset("pool", rw[:, 8 + n:16 + n], 0.0, w=[rwk])
                P.dma("sp", rw[:, 8:8 + n], g.ut[2048 + ct * 128:2048 + (ct + 1) * 128, tok0:tok0 + n], r=["d:ut"], w=[rwk])
                P.tt("dve", f1[:, 1:15 + n], rw[:, 0:14 + n], rw[:, 1:15 + n], ALU.add, r=[rwk], w=["f1"])
                res, resk = f1, "f1"
                if gi >= 1:
                    P.tt("pool", f2[:, 2:14 + n], f1[:, 1:13 + n], f1[:, 3:15 + n], ALU.add, r=["f1"], w=["f2"])
                    res, resk = f2, "f2"
                if gi >= 2:
                    P.tt("dve", f1[:, 4:12 + n], f2[:, 2:10 + n], f2[:, 6:14 + n], ALU.add, r=["f2"], w=["f1"])
                    res, resk = f1, "f1"
                if gi >= 3:
                    P.tt("pool", f2[:, 8:8 + n], f1[:, 4:4 + n], f1[:, 12:12 + n], ALU.add, r=["f1"], w=["f2"])
                    res, resk = f2, "f2"
                P.tt("dve", res[:, 8:8 + n], res[:, 8:8 + n], inv[:, 8:8 + n], ALU.mult, r=[resk, "inv"], w=[resk])
                P.tt("pool", PT[:, ct, tok0:tok0 + n], res[:, 8:8 + n], rw[:, 8:8 + n], ALU.subtract, r=[resk, rwk], w=[f"PT{ct}"])
    pps = Rot(P, "P_ps", [128, 512], F32, 2, psum=True)
    zr = Rot(P, "P_z", [128, 512], BF16, 2)
    er = Rot(P, "P_e", [128, 512], F32, 2)
    yr = Rot(P, "P_y", [128, 512], BF16, 2)
    for (t0, nt) in blocks():
        P.split()
        TB = nt * 128
        tok0 = t0 * 128
        for gi in range(4):
            for dt in range(2):
                idx = 2 * gi + dt
                ps, psk = pps.next()
                for cj in range(2):
                    P.mm(ps[:, 0:TB], pw[:, gi * 2 + cj, dt * 128:(dt + 1) * 128], PT[:, 2 * gi + cj, tok0:tok0 + TB], cj == 0, cj == 1,
                         r=["pw", f"PT{2 * gi + cj}"], w=[psk])
                z, zk = zr.next()
                e, ek = er.next()
                y, yk = yr.next()
                P.dma("sp", z[:, 0:TB], g.ut[3072 + idx * 128:3072 + (idx + 1) * 128, tok0:tok0 + TB], r=["d:ut"], w=[zk])
                P.act(e[:, 0:TB], z[:, 0:TB], AF.Exp, r=[zk], w=[ek], scale=-1.0)
                P.ts("pool", e[:, 0:TB], e[:, 0:TB], 1.0, ALU.add, r=[ek], w=[ek])
                P.recip(e[:, 0:TB], e[:, 0:TB], r=[ek], w=[ek])
                P.tt("pool", e[:, 0:TB], e[:, 0:TB], z[:, 0:TB], ALU.mult, r=[ek, zk], w=[ek])
                P.stt(y[:, 0:TB], ps[:, 0:TB], psc[:, idx:idx + 1], e[:, 0:TB], ALU.mult, ALU.mult, r=[psk, "psc", ek], w=[yk])
                P.dma("pool", g.ypt[idx * 128:(idx + 1) * 128, tok0:tok0 + TB], y[:, 0:TB], r=[yk], w=["d:ypt"])
    P.end_stage()


def stage_even_S(g, l, xin, xout, last):
    P = g.P
    j = l // 2
    P.begin_stage()
    wo = P.sb("S_wo", [128, 16, D], BF16)
    for k in range(16):
        P.dma("pool", wo[:, k, :], g.ab_w_out[g.J(j), k * 128:(k + 1) * 128, :], w=[f"wo{k}"])
    cw = P.sb("S_cw", [128, 48], F32)
    P.dma("sp", cw[:], g.ab_conv[g.J(j)], w=["cw"])
    bg = P.sb("S_bg", [128, 16], F32)
    P.dma("sp", bg[:], g.ab_b_gate[g.J(j), :].partition_broadcast(128), w=["bg"])
    mn = P.sb("S_mn", [128, D], F32)
    P.dma("sp", mn[:], g.ab_mnorm[g.J(j), :].partition_broadcast(128), w=["mn"])
    GG = P.sb("S_GG", [128, 2, D], F32)
    for s in range(2):
        P.dma("sp", GG[:, s, :], g.modv[l, s, 2, :].partition_broadcast(128), w=["GG"], stream=f"gg{s}")
    Wb = [[P.sb(f"S_Wb{a}{t}", [128, 8, 128], F32) for t in range(3)] for a in range(2)]
    cwv = cw[:].rearrange("p (c t) -> p c t", t=3)
    for a in range(2):
        for t in range(3):
            P.cp("dve", Wb[a][t][:], cwv[:, a * 8:(a + 1) * 8, t:t + 1].to_broadcast([128, 8, 128]), r=["cw"], w=[f"Wb{a}{t}"])
    C = P.sb("S_C", [128, 4, 2, 257], F32)
    Cb = P.sb("S_Cb", [128, 4, 2, 257], BF16)
    raws = [Rot(P, "S_qr", [128, 8, 130], BF16, 2), Rot(P, "S_kr", [128, 8, 130], BF16, 2)]
    cvt = [P.sb(f"S_cvt{i}", [128, 8, 128], F32) for i in range(3)]
    qkc = [Rot(P, "S_qc", [128, 8, 128], BF16, 2), Rot(P, "S_kc", [128, 8, 128], BF16, 2)]
    ktokr = Rot(P, "S_ktok", [128, D], BF16, 2)
    vtokr = Rot(P, "S_vtok", [128, 3072], BF16, 2)
    gtr = Rot(P, "S_gt", [128, 16], F32, 2)
    gw = Rot(P, "S_gw", [128, 48], F32, 2)
    lhr = Rot(P, "S_lh", [128, 8], BF16, 2)
    v1r = Rot(P, "S_v1", [128, 4, 257], BF16, 2)
    v2r = Rot(P, "S_v2", [128, 4, 257], BF16, 2)
    smr = Rot(P, "S_sm", [128, 128], BF16, 2)
    dnr = Rot(P, "S_dn", [128, 8], F32, 2)
    hhr = Rot(P, "S_hh", [128, D], F32, 2)
    hfr = Rot(P, "S_hf", [128, D], F32, 2)
    xr = Rot(P, "S_x", [128, D], F32, 2)
    ypr = Rot(P, "S_yp", [128, 8, 128], BF16, 2)
    t1 = P.sb("S_t1", [128, D], F32)
    t2 = P.sb("S_t2", [128, D], F32)
    t3 = P.sb("S_t3", [128, D], F32)
    st4 = Rot(P, "S_st4", [128, 16], F32, 2)
    ymr = Rot(P, "S_ym", [128, D], BF16, 2)
    ymTr = Rot(P, "S_ymT", [128, 8, 128], BF16, 2)
    xor_ = Rot(P, "S_xo", [128, D], F32, 2)
    p_tr = Rot(P, "S_ptr", [128, D], BF16, 1, psum=True)
    p_g = Rot(P, "S_pg", [128, 8], F32, 1, psum=True)
    p_s = Rot(P, "S_pst", [128, 128], F32, 1, psum=True)
    p_in = Rot(P, "S_pin", [128, 257], F32, 1, psum=True)
    p_c = [Rot(P, "S_pc0", [128, 257], F32, 1, psum=True), Rot(P, "S_pc1", [128, 257], F32, 1, psum=True)]
    p_wo = [Rot(P, "S_pw0", [128, 512], F32, 1, psum=True), Rot(P, "S_pw1", [128, 512], F32, 1, psum=True)]
    utq = [g.ut[0:1024, :].rearrange("(j p) t -> p j t", p=128), g.ut[1024:2048, :].rearrange("(j p) t -> p j t", p=128)]
    yptv = g.ypt.rearrange("(j p) t -> p j t", p=128)
    wokeys = [f"wo{k}" for k in range(16)]

    import os
    _dirs = tuple(int(c) for c in os.environ.get("KDIRS", "01"))
    _ntl = int(os.environ.get("KNT", "99"))
    _off = os.environ.get("KOFF", "")
    for dirn in _dirs:
        P.memset("pool", C[:], 0.0, w=["C"])
        P.memset("pool", Cb[:], 0.0, w=["Cb"])
        order = list(range(NTILE)) if dirn == 0 else [1, 0] + list(range(NTILE - 1, 1, -1))
        tri = g.trif if dirn == 0 else g.trib
        trib16 = g.trifb if dirn == 0 else g.tribb
        trik = "trif" if dirn == 0 else "trib"
        for tile in order[:_ntl]:
            P.split()
            isctx = tile < 2
            src = 1 if isctx else 0
            need_out = not (last and isctx)
            seq_lo = tile in (0, 2)
            seq_hi = tile in (1, NTILE - 1)
            tok = tile * 128
            c_lo = 1 if seq_lo else 0
            c_hi = 129 if seq_hi else 130
            conv = []
            for a in range(2):
                rw, rwk = raws[a].next()
                if seq_lo:
                    P.memset("pool", rw[:, :, 0:1], 0.0, w=[rwk])
                if seq_hi:
                    P.memset("pool", rw[:, :, 129:130], 0.0, w=[rwk])
                P.dma("sp", rw[:, :, c_lo:c_hi], utq[a][:, :, tok - 1 + c_lo:tok - 1 + c_hi], r=["d:ut"], w=[rwk])
                oc, ock = qkc[a].next()
                if a == 1 or need_out:
                    P.tt("pool", cvt[0][:], rw[:, :, 0:128], Wb[a][0][:], ALU.mult, r=[rwk, f"Wb{a}0"], w=["cvt0"])
                    P.tt("dve", cvt[1][:], rw[:, :, 1:129], Wb[a][1][:], ALU.mult, r=[rwk, f"Wb{a}1"], w=["cvt1"])
                    P.tt("pool", cvt[2][:], rw[:, :, 2:130], Wb[a][2][:], ALU.mult, r=[rwk, f"Wb{a}2"], w=["cvt2"])
                    P.tt("dve", cvt[0][:], cvt[0][:], cvt[1][:], ALU.add, r=["cvt0", "cvt1"], w=["cvt0"])
                    P.tt("pool", oc[:], cvt[0][:], cvt[2][:], ALU.add, r=["cvt0", "cvt2"], w=[ock])
                conv.append((oc, ock))
            (qc, qck), (kc, kck) = conv
            pt, ptk = p_tr.next()
            for ct in range(8):
                P.tr(pt[:, ct * 128:(ct + 1) * 128], kc[:, ct, :], g.ident[:], r=[kck, "ident"], w=[ptk])
            ktok, ktk = ktokr.next()
            P.cp("act", ktok[:], pt[:], r=[ptk], w=[ktk])
            vtok, vtk = vtokr.next()
            P.dma("sp", vtok[:], g.utok[tok:tok + 128, :], r=["d:utok"], w=[vtk])
            gtt, gtk = gtr.next()
            P.dma("sp", gtt[:], g.gt[tok:tok + 128, :], r=["d:gt"], w=[gtk])
            G, Gk = gw.next()
            gg = G[:, 0:16]
            l1, tmp, tmp2, ea, ea2, eb, edec, e1 = [G[:, 16 + 4 * i:20 + 4 * i] for i in range(8)]
            P.tt("pool", gg, gtt[:], bg[:], ALU.add, r=[gtk, "bg"], w=[Gk + "g"])
            li = G[:, 8 * dirn:8 * dirn + 4]
            fr = G[:, 8 * dirn + 4:8 * dirn + 8]
            P.act(e1, fr, AF.Exp, r=[Gk + "g"], w=[Gk + "e1"], scale=-1.0)
            P.act(l1, e1, AF.Ln, r=[Gk + "e1"], w=[Gk + "l1"], bias=g.cc[:, 1:2], scale=1.0)
            pg, pgk = p_g.next()
            lh, lhk = lhr.next()
            P.cp("dve", lh[:, 0:4], l1, r=[Gk + "l1"], w=[lhk])
            P.tt("dve", lh[:, 4:8], l1, lh[:, 0:4], ALU.subtract, r=[Gk + "l1", lhk], w=[lhk])
            P.mm(pg[:, 0:4], trib16[:], lh[:, 0:4], True, False, r=[trik + "b", lhk], w=[pgk])
            P.mm(pg[:, 0:4], trib16[:], lh[:, 4:8], False, True, r=[trik + "b", lhk], w=[pgk])
            P.mm(pg[:, 4:8], g.onesb[:], lh[:, 0:4], True, False, r=["onesb", lhk], w=[pgk])
            P.mm(pg[:, 4:8], g.onesb[:], lh[:, 4:8], False, True, r=["onesb", lhk], w=[pgk])
            P.tt("dve", tmp, li, pg[:, 0:4], ALU.add, r=[Gk + "g", pgk], w=[Gk + "tmp"])
            P.tt("dve", tmp2, tmp, pg[:, 4:8], ALU.subtract, r=[Gk + "tmp", pgk], w=[Gk + "tmp2"])
            P.act(ea, tmp, AF.Exp, r=[Gk + "tmp"], w=[Gk + "ea"], bias=g.cc[:, 3:4], scale=1.0)
            P.act(ea2, tmp2, AF.Exp, r=[Gk + "tmp2"], w=[Gk + "ea2"], bias=g.cc[:, 3:4], scale=1.0)
            P.act(eb, pg[:, 0:4], AF.Exp, r=[pgk], w=[Gk + "eb"], scale=-1.0)
            P.act(edec, pg[:, 4:8], AF.Exp, r=[pgk], w=[Gk + "edec"], scale=-1.0)
            v1, v1k = v1r.next()
            v2, v2k = v2r.next()
            for h in range(4):
                if need_out:
                    P.ts("dve", v1[:, h, 0:256], vtok[:, h * 256:(h + 1) * 256], ea[:, h:h + 1], ALU.mult, r=[vtk, Gk + "ea"], w=[v1k])
                P.ts("pool", v2[:, h, 0:256], vtok[:, h * 256:(h + 1) * 256], ea2[:, h:h + 1], ALU.mult, r=[vtk, Gk + "ea2"], w=[v2k])
            if need_out:
                P.cp("dve", v1[:, :, 256], ea, r=[Gk + "ea"], w=[v1k])
            P.cp("pool", v2[:, :, 256], ea2, r=[Gk + "ea2"], w=[v2k])
            hh, hhk = hhr.next()
            for h in range(4):
                if need_out:
                    ps_, psk_ = p_s.next()
                    for jj in range(2):
                        P.mm(ps_[:], kc[:, 2 * h + jj, :], qc[:, 2 * h + jj, :], jj == 0, jj == 1, r=[kck, qck], w=[psk_])
                    sm, smk = smr.next()
                    P.tt("dve", sm[:], ps_[:], tri[:], ALU.mult, r=[psk_, trik], w=[smk])
                    pi, pik = p_in.next()
                    for jj in range(2):
                        P.mm(pi[:], qc[:, 2 * h + jj, :], Cb[:, h, jj, :], jj == 0, False, r=[qck, "Cb"], w=[pik])
                    P.mm(pi[:], sm[:], v1[:, h, :], False, True, r=[smk, v1k], w=[pik])
                    dn, dnk = dnr.next()
                    P.ts("dve", dn[:, 0:1], pi[:, 256:257], eb[:, h:h + 1], ALU.mult, r=[pik, Gk + "eb"], w=[dnk])
                    P.stt(dn[:, 1:2], dn[:, 0:1], -1.0, dn[:, 0:1], ALU.mult, ALU.max, r=[dnk], w=[dnk])
                    P.ts("dve", dn[:, 2:3], dn[:, 1:2], 1.0, ALU.max, r=[dnk], w=[dnk])
                    P.recip(dn[:, 3:4], dn[:, 2:3], r=[dnk], w=[dnk])
                    P.tt("dve", dn[:, 4:5], dn[:, 3:4], eb[:, h:h + 1], ALU.mult, r=[dnk, Gk + "eb"], w=[dnk])
                    P.ts("dve", hh[:, h * 256:(h + 1) * 256], pi[:, 0:256], dn[:, 4:5], ALU.mult, r=[pik, dnk], w=[hhk])
                for jj in range(2):
                    pc, pck = p_c[jj].next()
                    P.mm(pc[:], ktok[:, h * 256 + jj * 128:h * 256 + (jj + 1) * 128], v2[:, h, :], True, True, r=[ktk, v2k], w=[pck])
                    P.stt(C[:, h, jj, :], C[:, h, jj, :], edec[:, h:h + 1], pc[:], ALU.mult, ALU.add, r=["C", Gk + "edec", pck], w=["C"])
                    P.cp("pool", Cb[:, h, jj, :], C[:, h, jj, :], r=["C"], w=["Cb"])
            if not need_out:
                continue
            if dirn == 0:
                P.dma("pool", g.hf[tok:tok + 128, :], hh[:], r=[hhk], w=["d:hf"])
                continue
            hf, hfk = hfr.next()
            P.dma("sp", hf[:], g.hf[tok:tok + 128, :], r=["d:hf"], w=[hfk])
            xt, xk = xr.next()
            P.dma("sp", xt[:], xin[tok:tok + 128, :], r=["d:xin"], w=[xk])
            yp, ypk = ypr.next()
            P.dma("sp", yp[:], yptv[:, :, tok:tok + 128], r=["d:ypt"], w=[ypk])
            P.tt("pool", hh[:], hh[:], hf[:], ALU.add, r=[hhk, hfk], w=[hhk])
            P.act(t1[:], vtok[:, 1024:2048], AF.Exp, r=[vtk], w=["t1"], scale=-1.0)
            P.ts("pool", t1[:], t1[:], 1.0, ALU.add, r=["t1"], w=["t1"])
            P.recip(t1[:], t1[:], r=["t1"], w=["t1"])
            P.tt("pool", hh[:], hh[:], t1[:], ALU.mult, r=[hhk, "t1"], w=[hhk])
            s4, s4k = st4.next()
            for h in range(4):
                P.act(t1[:, h * 256:(h + 1) * 256], hh[:, h * 256:(h + 1) * 256], AF.Square, r=[hhk], w=["t1", s4k],
                      accum=s4[:, h:h + 1])
            P.act(s4[:, 4:8], s4[:, 0:4], AF.Ln, r=[s4k], w=[s4k], bias=g.cc[:, 2:3], scale=1.0 / 256.0)
            P.act(s4[:, 8:12], s4[:, 4:8], AF.Exp, r=[s4k], w=[s4k], scale=-0.5)
            P.act(t2[:], vtok[:, 2048:3072], AF.Exp, r=[vtk], w=["t2"], scale=-1.0)
            P.ts("pool", t2[:], t2[:], 1.0, ALU.add, r=["t2"], w=["t2"])
            P.recip(t2[:], t2[:], r=["t2"], w=["t2"])
            P.tt("pool", t2[:], t2[:], vtok[:, 2048:3072], ALU.mult, r=["t2", vtk], w=["t2"])
            P.tt("pool", t2[:], t2[:], mn[:], ALU.mult, r=["t2", "mn"], w=["t2"])
            ym, ymk = ymr.next()
            for h in range(4):
                P.stt(ym[:, h * 256:(h + 1) * 256], hh[:, h * 256:(h + 1) * 256], s4[:, 8 + h:9 + h], t2[:, h * 256:(h + 1) * 256],
                      ALU.mult, ALU.mult, r=[hhk, s4k, "t2"], w=[ymk])
            pt, ptk = p_tr.next()
            for ct in range(8):
                P.tr(pt[:, ct * 128:(ct + 1) * 128], ym[:, ct * 128:(ct + 1) * 128], g.ident[:], r=[ymk, "ident"], w=[ptk])
            ymT, ymTk = ymTr.next()
            P.cp("act", ymT[:], pt[:].rearrange("p (k t) -> p k t", k=8), r=[ptk], w=[ymTk])
            pws = []
            for nt_ in range(2):
                pw_, pwk_ = p_wo[nt_].next()
                for f in range(16):
                    lhs = ymT[:, f, :] if f < 8 else yp[:, f - 8, :]
                    lk = ymTk if f < 8 else ypk
                    P.mm(pw_[:], lhs, wo[:, f, nt_ * 512:(nt_ + 1) * 512], f == 0, f == 15, r=[lk, wokeys[f]], w=[pwk_])
                pws.append((pw_, pwk_))
            post_norm_residual(g, pws, xt, xk, GG, src, t3, st4, xor_, xout, tile, last)
    P.end_stage()


def post_norm_residual(g, pws, xt, xk, GG, src, t3, st4, xor_, xout, tile, last):
    P = g.P
    s5, s5k = st4.next()
    for nt_ in range(2):
        pw_, pwk_ = pws[nt_]
        P.act(t3[:, nt_ * 512:(nt_ + 1) * 512], pw_[:], AF.Square, r=[pwk_], w=["t3", s5k], accum=s5[:, nt_:nt_ + 1])
    P.tt("dve", s5[:, 2:3], s5[:, 0:1], s5[:, 1:2], ALU.add, r=[s5k], w=[s5k])
    P.act(s5[:, 3:4], s5[:, 2:3], AF.Ln, r=[s5k], w=[s5k], bias=g.cc[:, 2:3], scale=1.0 / D)
    P.act(s5[:, 4:5], s5[:, 3:4], AF.Exp, r=[s5k], w=[s5k], scale=-0.5)
    xo, xok = xor_.next()
    for nt_ in range(2):
        pw_, pwk_ = pws[nt_]
        P.stt(t3[:, nt_ * 512:(nt_ + 1) * 512], pw_[:], s5[:, 4:5], GG[:, src, nt_ * 512:(nt_ + 1) * 512], ALU.mult, ALU.mult,
              r=[pwk_, s5k, "GG"], w=["t3"])
    P.tt("pool", xo[:], t3[:], xt[:], ALU.add, r=["t3", xk], w=[xok])
    tok = tile * 128
    if last:
        P.dma("pool", g.out[tok - NCTX:tok - NCTX + 128, :], xo[:], r=[xok], w=["d:xout"])
    else:
        P.dma("pool", xout[tok:tok + 128, :], xo[:], r=[xok], w=["d:xout"])


def stage_odd(g, l, xin, xout, last):
    P = g.P
    j = l // 2
    P.begin_stage()
    kT = P.sb("O_kT", [128, 4, NT], BF16)
    va = P.sb("O_va", [128, NTILE, 4, 65], BF16)
    P.memset("pool", va[:, :, :, 64:65], 1.0, w=["va_ones"])
    P.begin_stage()
    w = P.sb("OA_w", [128, 8, C_IN], BF16)
    for k in range(8):
        P.dma("pool", w[:, k, :], g.c_w_in[g.J(j), k * 128:(k + 1) * 128, :], w=[f"wC{k}"])
    wkeys = [f"wC{k}" for k in range(8)]
    A = P.sb("OA_A", [128, 2, D], F32)
    B = P.sb("OA_B", [128, 2, D], F32)
    load_AB(g, l, A, B)
    xr = Rot(P, "OA_x", [128, D], F32, 2)
    junk = P.sb("OA_junk", [128, D], F32)
    stat = Rot(P, "OA_stat", [128, 4], F32, 2)
    hbr = Rot(P, "OA_hb", [128, D], BF16, 2)
    hTr = Rot(P, "OA_hT", [128, 8, 128], BF16, 2)
    rpr = Rot(P, "OA_rp", [128, 64], F32, 2)
    tmr = [P.sb(f"OA_tm{i}", [128, 8, 32], F32) for i in range(4)]
    qrr = Rot(P, "OA_qr", [128, 16, 64], BF16, 2)
    kdr = Rot(P, "OA_kd", [128, 4, 2, 64], BF16, 2)
    qTr = Rot(P, "OA_qT", [128, 8, 128], BF16, 2)
    zbr = Rot(P, "OA_zb", [128, D], BF16, 2)
    p_h = Rot(P, "OA_ph", [128, D], BF16, 1, psum=True)
    p_u = Rot(P, "OA_pu", [128, 512], F32, 3, psum=True)
    p_q = Rot(P, "OA_pq", [128, D], BF16, 1, psum=True)
    p_k = Rot(P, "OA_pk", [128, 512], BF16, 1, psum=True)
    for tile in range(NTILE):
        P.split()
        src = 1 if tile < 2 else 0
        tok = tile * 128
        xt, xk = xr.next()
        st, sk = stat.next()
        hb, hbk = hbr.next()
        P.dma("sp", xt[:], xin[tok:tok + 128, :], r=["d:xin"], w=[xk])
        rp, rpk = rpr.next()
        P.dma("sp", rp[:], g.k_rope[tok:tok + 128, :], w=[rpk])
        norm_mod_tile(g, sk, xt, xk, A, B, src, g.cc, hb, hbk, junk, st)
        pt, ptk = p_h.next()
        for k in range(8):
            P.tr(pt[:, k * 128:(k + 1) * 128], hb[:, k * 128:(k + 1) * 128], g.ident[:], r=[hbk, "ident"], w=[ptk])
        hT, hTk = hTr.next()
        P.cp("act", hT[:], pt[:].rearrange("p (k t) -> p k t", k=8), r=[ptk], w=[hTk])
        qr, qrk = qrr.next()
        kd, kdk = kdr.next()
        zb, zbk = zbr.next()
        for n in range(5):
            ps, psk = p_u.next()
            for k in range(8):
                P.mm(ps[:], hT[:, k, :], w[:, k, n * 512:(n + 1) * 512], k == 0, k == 7, r=[hTk, wkeys[k]], w=[psk])
            if n < 2 or n == 2:
                nh = 8 if n < 2 else 4
                pv = ps[:, 0:nh * 64].rearrange("p (h i two) -> p h i two", h=nh, two=2)
                x1 = pv[:, :, :, 0]
                x2 = pv[:, :, :, 1]
                cosb = rp[:, 0:32].unsqueeze(1).to_broadcast([128, nh, 32])
                sinb = rp[:, 32:64].unsqueeze(1).to_broadcast([128, nh, 32])
                tm = [t[:, 0:nh, :] for t in tmr]
                P.tt("dve", tm[0], x1, cosb, ALU.mult, r=[psk, rpk], w=["tm0"])
                P.tt("dve", tm[1], x2, sinb, ALU.mult, r=[psk, rpk], w=["tm1"])
                P.tt("dve", tm[2], x1, sinb, ALU.mult, r=[psk, rpk], w=["tm2"])
                P.tt("dve", tm[3], x2, cosb, ALU.mult, r=[psk, rpk], w=["tm3"])
                if n < 2:
                    P.tt("pool", qr[:, 8 * n:8 * n + 8, 0:32], tm[0], tm[1], ALU.subtract, r=["tm0", "tm1"], w=[qrk])
                    P.tt("pool", qr[:, 8 * n:8 * n + 8, 32:64], tm[2], tm[3], ALU.add, r=["tm2", "tm3"], w=[qrk])
                else:
                    for c in range(2):
                        P.tt("pool", kd[:, :, c, 0:32], tm[0], tm[1], ALU.subtract, r=["tm0", "tm1"], w=[kdk])
                        P.tt("pool", kd[:, :, c, 32:64], tm[2], tm[3], ALU.add, r=["tm2", "tm3"], w=[kdk])
                    P.cp("act", va[:, tile, :, 0:64], ps[:, 256:512].rearrange("p (g d) -> p g d", g=4), r=[psk], w=[f"va{tile}"])
            else:
                P.cp("act", zb[:, (n - 3) * 512:(n - 2) * 512], ps[:], r=[psk], w=[zbk])
        P.dma("pool", g.zt[tok:tok + 128, :], zb[:], r=[zbk], w=["d:zt"])
        pq, pqk = p_q.next()
        qrf = qr[:].rearrange("p h d -> p (h d)")
        for k in range(8):
            P.tr(pq[:, k * 128:(k + 1) * 128], qrf[:, k * 128:(k + 1) * 128], g.ident[:], r=[qrk, "ident"], w=[pqk])
        qT, qTk = qTr.next()
        P.cp("dve", qT[:], pq[:].rearrange("p (k t) -> p k t", k=8), r=[pqk], w=[qTk])
        P.dma("pool", g.qt[tile], qT[:], r=[qTk], w=["d:qt"])
        pk, pkk = p_k.next()
        kdf = kd[:].rearrange("p g c d -> p (g c d)")
        for gq in range(4):
            P.tr(pk[:, gq * 128:(gq + 1) * 128], kdf[:, gq * 128:(gq + 1) * 128], g.ident[:], r=[kdk, "ident"], w=[pkk])
        P.cp("act", kT[:, :, tok:tok + 128], pk[:].rearrange("p (g t) -> p g t", g=4), r=[pkk], w=[f"kT{tile}"])
    P.end_stage()
    P.begin_stage()
    wo = P.sb("OB_wo", [128, 8, D], BF16)
    for k in range(8):
        P.dma("pool", wo[:, k, :], g.c_w_out[g.J(j), k * 128:(k + 1) * 128, :], w=[f"woC{k}"])
    snk = P.sb("OB_snk", [128, 16], F32)
    nsnk = P.sb("OB_nsnk", [128, 16], F32)
    P.dma("sp", snk[:], g.c_sink[g.J(j), :].partition_broadcast(128), w=["snk"])
    P.ts("dve", nsnk[:], snk[:], -1.0, ALU.mult, r=["snk"], w=["nsnk"])
    GG = P.sb("OB_GG", [128, 2, D], F32)
    for s_ in range(2):
        P.dma("sp", GG[:, s_, :], g.modv[l, s_, 2, :].partition_broadcast(128), w=["GG"], stream=f"ogg{s_}")
    qTr = Rot(P, "OB_qT", [128, 8, 128], BF16, 2)
    zr = Rot(P, "OB_z", [128, D], BF16, 2)
    xr = Rot(P, "OB_x", [128, D], F32, 2)
    mxr = Rot(P, "OB_mx", [128, 8], F32, 3)
    prr = Rot(P, "OB_pr", [128, 5, 128], BF16, 2)
    ptsr = Rot(P, "OB_pts", [128, 5, 128], BF16, 2)
    orr = Rot(P, "OB_o", [128, D], F32, 2)
    t1 = P.sb("OB_t1", [128, D], F32)
    t3 = P.sb("OB_t3", [128, D], F32)
    ogr = Rot(P, "OB_og", [128, D], BF16, 2)
    ogTr = Rot(P, "OB_ogT", [128, 8, 128], BF16, 2)
    st4 = Rot(P, "OB_st", [128, 16], F32, 2)
    xor_ = Rot(P, "OB_xo", [128, D], F32, 2)
    p_sc = Rot(P, "OB_psc", [128, 2, 512], F32, 1, psum=True)
    p_pt = Rot(P, "OB_ppt", [128, 5, 128], BF16, 1, psum=True)
    p_oa = Rot(P, "OB_poa", [128, 65], F32, 2, psum=True)
    p_og = Rot(P, "OB_pog", [128, D], BF16, 1, psum=True)
    p_wo = [Rot(P, "OB_pw0", [128, 512], F32, 1, psum=True), Rot(P, "OB_pw1", [128, 512], F32, 1, psum=True)]
    wokeys = [f"woC{k}" for k in range(8)]
    qtiles = list(range(2, NTILE)) + ([] if last else [0, 1])
    tog = 0
    for tile in qtiles:
        P.split()
        isctx = tile < 2
        src = 1 if isctx else 0
        tok = tile * 128
        band = [] if isctx else [t for t in (tile - 1, tile, tile + 1) if 2 <= t < NTILE]
        nbt = len(band)
        nkb = 2 + nbt
        qT, qTk = qTr.next()
        P.dma("sp", qT[:], g.qt[tile], r=["d:qt"], w=[qTk])
        z, zk = zr.next()
        P.dma("sp", z[:], g.zt[tok:tok + 128, :], r=["d:zt"], w=[zk])
        xt, xk = xr.next()
        P.dma("sp", xt[:], xin[tok:tok + 128, :], r=["d:xin"], w=[xk])
        o, ok_ = orr.next()
        for h in range(16):
            g_ = h // 4
            pb = (h % 2) * 64
            qtl = h // 2
            sc, sck = p_sc.next()
            P.mm(sc[:, 0, 0:256], qT[pb:pb + 64, qtl, :], kT[pb:pb + 64, g_, 0:256], True, True, r=[qTk], w=[sck])
            if nbt:
                b0 = band[0] * 128
                P.mm(sc[:, 1, 0:nbt * 128], qT[pb:pb + 64, qtl, :], kT[pb:pb + 64, g_, b0:b0 + nbt * 128], True, True, r=[qTk], w=[sck])
            mx, mxk = mxr.next()
            P.op("dve", lambda e, mx=mx, sc=sc: e.tensor_reduce(out=mx[:, 0:1], in_=sc[:, 0, 0:256], axis=AX.X, op=ALU.max), r=[sck], w=[mxk])
            if nbt:
                P.op("dve", lambda e, mx=mx, sc=sc, nbt=nbt: e.tensor_reduce(out=mx[:, 1:2], in_=sc[:, 1, 0:nbt * 128], axis=AX.X, op=ALU.max), r=[sck], w=[mxk])
                P.tt("dve", mx[:, 2:3], mx[:, 0:1], mx[:, 1:2], ALU.max, r=[mxk], w=[mxk])
                msrc = mx[:, 2:3]
            else:
                msrc = mx[:, 0:1]
            P.ts("dve", mx[:, 3:4], msrc, -0.125, ALU.mult, r=[mxk, "nsnk"], w=[mxk], s2=nsnk[:, h:h + 1], op1=ALU.min)
            pr, prk = prr.next()
            P.act(pr[:, 0:2, :], sc[:, 0, 0:256].rearrange("p (b s) -> p b s", b=2), AF.Exp, r=[sck, mxk], w=[prk], bias=mx[:, 3:4], scale=0.125)
            if nbt:
                P.act(pr[:, 2:2 + nbt, :], sc[:, 1, 0:nbt * 128].rearrange("p (b s) -> p b s", b=nbt), AF.Exp, r=[sck, mxk], w=[prk],
                      bias=mx[:, 3:4], scale=0.125)
            P.act(mx[:, 4:5], snk[:, h:h + 1], AF.Exp, r=["snk", mxk], w=[mxk], bias=mx[:, 3:4], scale=1.0)
            for bi, t in enumerate(band):
                if t == tile - 1:
                    P.tt("pool", pr[:, 2 + bi, :], pr[:, 2 + bi, :], g.trifb[:], ALU.mult, r=[prk, "trifb"], w=[prk])
                elif t == tile + 1:
                    P.tt("pool", pr[:, 2 + bi, :], pr[:, 2 + bi, :], g.tribb[:], ALU.mult, r=[prk, "tribb"], w=[prk])
            ptp, ptpk = p_pt.next()
            for b in range(nkb):
                P.tr(ptp[:, b, :], pr[:, b, :], g.ident[:], r=[prk, "ident"], w=[ptpk])
            pts, ptsk = ptsr.next()
            eng = "act" if tog % 2 == 0 else "dve"
            tog += 1
            P.cp(eng, pts[:, 0:nkb, :], ptp[:, 0:nkb, :], r=[ptpk], w=[ptsk])
            oa, oak = p_oa.next()
            for b, kt in enumerate([0, 1] + band):
                P.mm(oa[:], pts[:, b, :], va[:, kt, g_, :], b == 0, b == nkb - 1, r=[ptsk], w=[oak])
            P.tt("dve", mx[:, 5:6], oa[:, 64:65], mx[:, 4:5], ALU.add, r=[oak, mxk], w=[mxk])
            P.recip(mx[:, 6:7], mx[:, 5:6], r=[mxk], w=[mxk])
            P.ts("dve", o[:, h * 64:(h + 1) * 64], oa[:, 0:64], mx[:, 6:7], ALU.mult, r=[oak, mxk], w=[ok_])
        P.act(t1[:], z[:], AF.Exp, r=[zk], w=["t1"], scale=-1.0)
        P.ts("pool", t1[:], t1[:], 1.0, ALU.add, r=["t1"], w=["t1"])
        P.recip(t1[:], t1[:], r=["t1"], w=["t1"])
        P.tt("pool", t1[:], t1[:], z[:], ALU.mult, r=["t1", zk], w=["t1"])
        og, ogk = ogr.next()
        P.tt("pool", og[:], o[:], t1[:], ALU.mult, r=[ok_, "t1"], w=[ogk])
        pg_, pgk_ = p_og.next()
        for k in range(8):
            P.tr(pg_[:, k * 128:(k + 1) * 128], og[:, k * 128:(k + 1) * 128], g.ident[:], r=[ogk, "ident"], w=[pgk_])
        ogT, ogTk = ogTr.next()
        P.cp("act", ogT[:], pg_[:].rearrange("p (k t) -> p k t", k=8), r=[pgk_], w=[ogTk])
        pws = []
        for nt_ in range(2):
            pw_, pwk_ = p_wo[nt_].next()
            for f in range(8):
                P.mm(pw_[:], ogT[:, f, :], wo[:, f, nt_ * 512:(nt_ + 1) * 512], f == 0, f == 7, r=[ogTk, wokeys[f]], w=[pwk_])
            pws.append((pw_, pwk_))
        post_norm_residual(g, pws, xt, xk, GG, src, t3, st4, xor_, xout, tile, last)
    P.end_stage()
    P.end_stage()


def build_program(layers=(0, 1, 2, 3), debug=False, stages=None, single=False):
    P = Prog()
    g = declare_io(P, layers, debug, single)
    load_consts(g)
    xin = g.xc
    for l in layers:
        last = l == 3
        xout = g.xcs[l % 2]
        stage_mod(g, l)
        if l % 2 == 0:
            if stages is None or "A" in stages:
                stage_even_A(g, l, xin)
            if stages is None or "P" in stages:
                stage_even_P(g, l)
            if stages is None or "S" in stages:
                stage_even_S(g, l, xin, xout, last)
        else:
            stage_odd(g, l, xin, xout, last)
        xin = xout
    nc = P.build()
    return P, nc, g


def _const_tables():
    ident = np.eye(128, dtype=np.float32)
    s = np.arange(128)[:, None]
    t = np.arange(128)[None, :]
    trif = (s <= t).astype(np.float32)
    trib = (s >= t).astype(np.float32)
    inv = np.zeros((8, NX + 16), np.float32)
    for gi, wdw in enumerate((2, 4, 8, 16)):
        h = wdw // 2
        for row, n in ((gi, NX), (4 + gi, NCTX)):
            tt = np.arange(n)
            cnt = np.minimum(tt + h, n) - np.maximum(tt - h, 0)
            inv[row, 8:8 + n] = 1.0 / cnt
    rope = np.zeros((NT, 64), np.float32)
    rope[:NCTX, :32] = 1.0
    rows = NX // 64
    row = np.repeat(np.arange(rows), 64).astype(np.float32)
    col = np.tile(np.arange(64), rows).astype(np.float32)
    nf = 16
    invf = (10000.0 ** (-np.arange(nf, dtype=np.float32) / nf)).astype(np.float32)
    ang = np.concatenate([row[:, None] * invf, col[:, None] * invf], -1).astype(np.float32)
    rope[NCTX:, :32] = np.cos(ang)
    rope[NCTX:, 32:] = np.sin(ang)
    return dict(k_ident=ident, k_trif=trif, k_trib=trib, k_invcnt=inv, k_rope=rope)


def make_in_maps(inp, layer=None, xcs=None):
    f = lambda a: np.ascontiguousarray(np.asarray(a, dtype=np.float32))
    c, c_ctx = f(inp["c"]), f(inp["c_ctx"])
    ls = slice(None) if layer is None else slice(layer, layer + 1)
    js = slice(None) if layer is None else slice(layer // 2, layer // 2 + 1)
    even = layer is None or layer % 2 == 0
    odd = layer is None or layer % 2 == 1
    kt = _const_tables()
    shared = dict(w_mod=f(inp["w_mod"][ls]), b_mod=f(inp["b_mod"][ls]), g_pre=f(inp["g_pre"][ls]), g_post=f(inp["g_post"][ls]),
                  k_ident=kt["k_ident"], k_trif=kt["k_trif"], k_trib=kt["k_trib"])
    if even:
        shared.update(
            ab_w_in=f(inp["ab_w_in"][js]), ab_b_gate=f(inp["ab_b_gate"][js]),
            ab_conv=f(np.asarray(inp["ab_conv"]).reshape(2, 3, 16, 128).transpose(0, 3, 2, 1).reshape(2, 128, 48)[js]),
            ab_mnorm=f(inp["ab_mnorm"][js]), ab_pool_w=f(inp["ab_pool_w"][js]),
            ab_pool_scale=f(np.asarray(inp["ab_pool_scale"]).reshape(2, 8, 128).transpose(0, 2, 1)[js]),
            ab_w_out=f(inp["ab_w_out"][js]), k_invcnt=kt["k_invcnt"])
    if odd:
        shared.update(c_w_in=f(inp["c_w_in"][js]), c_sink=f(inp["c_sink"][js]), c_w_out=f(inp["c_w_out"][js]), k_rope=kt["k_rope"])
    maps = []
    for b in range(8):
        m = dict(shared)
        if xcs is None:
            m["xc"] = np.ascontiguousarray(np.concatenate([np.asarray(inp["ctx"][b], np.float32), np.asarray(inp["x"][b], np.float32)], axis=0))
        else:
            m["xc"] = np.ascontiguousarray(xcs[b])
        cs = np.zeros((128, 8, 2), np.float32)
        cs[:, :, 0] = c[b].reshape(8, 128).T
        cs[:, :, 1] = c_ctx.reshape(8, 128).T
        m["cs"] = np.ascontiguousarray(cs.reshape(128, 16))
        maps.append(m)
    return maps


def kernel(**inputs):
    P, nc, g = build_program(layers=(0, 1, 2, 3), debug=())
    maps = make_in_maps(inputs)
    outs = []
    for b in range(8):
        res = run_bass_kernel_spmd(nc, [maps[b]], core_ids=[0])
        outs.append(np.asarray(res.results[0]["out"], dtype=np.float32))
    return np.stack(outs, axis=0)
```

```python
import math
import numpy as np
from contextlib import ExitStack
import concourse.bass as bass
import concourse.mybir as mybir
from concourse.alu_op_type import AluOpType as ALU
from concourse.bass_utils import run_bass_kernel_spmd

F32 = mybir.dt.float32
BF16 = mybir.dt.bfloat16
AF = mybir.ActivationFunctionType
AX = mybir.AxisListType

D = 1024
NCTX = 256
NX = 4096
NT = NCTX + NX
NTILE = NT // 128
AB_IN = 7184
GATE_OFF = 7168
C_IN = 2560
EPS = 1e-6
LN16 = math.log(1.0 / 16.0)


class Op:
    __slots__ = ("eng", "fn", "deps", "flag", "cnt", "stream")


class Prog:
    ENG = ("pe", "act", "dve", "pool", "sp")

    def __init__(self):
        self.nc = bass.Bass("TRN2", target_bir_lowering=False)
        self.es = ExitStack()
        self.ops = {e: [] for e in self.ENG}
        self.lastw = {}
        self.readers = {}
        self.streams = {}
        self.stage_stack = []
        self.stage_id = 0
        self.slot_of = {}
        self.nslot_stage = {}

    def sb(self, name, shape, dt):
        es = self.stage_stack[-1] if self.stage_stack else self.es
        self.uid = getattr(self, "uid", 0) + 1
        return es.enter_context(self.nc.sbuf_tensor(f"{name}_u{self.uid}", list(shape), dt))

    def ps(self, name, shape, dt):
        es = self.stage_stack[-1] if self.stage_stack else self.es
        self.uid = getattr(self, "uid", 0) + 1
        return es.enter_context(self.nc.psum_tensor(f"{name}_u{self.uid}", list(shape), dt))

    def dram(self, name, shape, dt, kind="Internal"):
        return self.nc.dram_tensor(name, list(shape), dt, kind=kind).ap()

    def begin_stage(self):
        self.stage_stack.append(ExitStack())

    def end_stage(self):
        self.barrier()
        self.split()
        self.stage_stack.pop().close()

    def op(self, eng, fn, r=(), w=(), dma=None):
        if dma is not None:
            sk = (self.stage_id, dma)
            if sk not in self.slot_of:
                n = self.nslot_stage.get(self.stage_id, 0)
                self.slot_of[sk] = n
                self.nslot_stage[self.stage_id] = n + 1
            dma = ("slot", self.slot_of[sk])
        o = Op()
        o.eng, o.fn, o.flag, o.stream, o.cnt = eng, fn, False, dma, 0
        deps = []
        for k in r:
            d = self.lastw.get(k)
            if d is not None:
                deps.append(d)
        for k in w:
            d = self.lastw.get(k)
            if d is not None:
                deps.append(d)
            deps.extend(self.readers.get(k, ()))
        ded = []
        seen = set()
        for d in deps:
            if id(d) in seen:
                continue
            seen.add(id(d))
            if d.stream is None and dma is None and d.eng == "pe" and eng == "pe":
                continue
            ded.append(d)
        o.deps = ded
        for d in ded:
            d.flag = True
        for k in r:
            lst = self.readers.setdefault(k, [])
            if dma is None:
                lst[:] = [x for x in lst if not (x.stream is None and x.eng == eng)]
            lst.append(o)
        for k in w:
            self.lastw[k] = o
            self.readers[k] = []
        self.ops[eng].append(o)
        if dma is not None:
            self.streams.setdefault(dma, []).append(o)
        return o

    def dma(self, eng, out, in_, r=(), w=(), stream=None):
        if stream is None:
            ks = [k for k in list(w) + list(r) if not (isinstance(k, str) and k.startswith("d:"))]
            stream = ("st", ks[0])
        return self.op(eng, lambda e: e.dma_start(out=out, in_=in_), r=r, w=w, dma=stream)

    def split(self):
        for e in self.ENG:
            o = Op()
            o.eng, o.fn, o.flag, o.stream, o.cnt = e, "SPLIT", False, None, 0
            o.deps = []
            self.ops[e].append(o)

    def barrier(self):
        lasts = []
        for e in self.ENG:
            for o in reversed(self.ops[e]):
                if o.stream is None and o.fn is not None and o.fn != "SPLIT":
                    lasts.append(o)
                    break
        for s, lst in self.streams.items():
            lasts.append(lst[-1])
        for d in lasts:
            d.flag = True
        for e in self.ENG:
            o = Op()
            o.eng, o.fn, o.flag, o.stream, o.cnt = e, None, False, None, 0
            o.deps = list(lasts)
            self.ops[e].append(o)
        self.lastw = {}
        self.readers = {}
        self.stage_id += 1

    def build(self):
        nc = self.nc
        self.barrier()
        sem = {e: self.es.enter_context(nc.semaphore(f"s_{e}")) for e in self.ENG}
        ssem = {}
        for i, s in enumerate(self.streams):
            ssem[s] = self.es.enter_context(nc.semaphore(f"d_{i}"))
        for e in self.ENG:
            c = 0
            for o in self.ops[e]:
                if o.stream is None and o.fn is not None and o.fn != "SPLIT":
                    if o.flag:
                        c += 1
                    o.cnt = c
        for s, lst in self.streams.items():
            for i, o in enumerate(lst):
                o.cnt = 16 * (i + 1)
        self.maxcnt = max([0] + [o.cnt for e in self.ENG for o in self.ops[e]])
        self.ninstr = sum(len(v) for v in self.ops.values())

        segs = {e: [[]] for e in self.ENG}
        for e in self.ENG:
            for o in self.ops[e]:
                if o.fn == "SPLIT":
                    segs[e].append([])
                else:
                    segs[e][-1].append(o)
        nseg = len(segs["pe"])
        seen_all = {e: {} for e in self.ENG}

        def run(e, eng, si):
            seen = seen_all[e]
            for o in segs[e][si]:
                need = {}
                for d in o.deps:
                    key = ("s", d.stream) if d.stream is not None else ("e", d.eng)
                    if d.cnt > need.get(key, 0):
                        need[key] = d.cnt
                for key, v in need.items():
                    if seen.get(key, 0) < v:
                        sm = ssem[key[1]] if key[0] == "s" else sem[key[1]]
                        eng.wait_ge(sm, v)
                        seen[key] = v
                if o.fn is None:
                    continue
                ins = o.fn(eng)
                if o.stream is not None:
                    ins.then_inc(ssem[o.stream], 16)
                elif o.flag:
                    ins.then_inc(sem[e], 1)

        for si in range(nseg):
            if not any(segs[e][si] for e in self.ENG):
                continue
            with nc.Block() as block:
                @block.tensor
                def _(eng):
                    run("pe", eng, si)

                @block.scalar
                def _(eng):
                    run("act", eng, si)

                @block.vector
                def _(eng):
                    run("dve", eng, si)

                @block.gpsimd
                def _(eng):
                    run("pool", eng, si)

                @block.sync
                def _(eng):
                    run("sp", eng, si)
        self.es.close()
        return nc

    def act(self, out, in_, func, r, w, bias=None, scale=None, accum=None):
        kw = {}
        if bias is not None:
            kw["bias"] = bias
        if scale is not None:
            kw["scale"] = scale
        if accum is not None:
            kw["accum_out"] = accum
        return self.op("act", lambda e: e.activation(out=out, in_=in_, func=func, **kw), r=r, w=w)

    def tt(self, eng, out, in0, in1, op, r, w):
        return self.op(eng, lambda e: e.tensor_tensor(out=out, in0=in0, in1=in1, op=op), r=r, w=w)

    def ts(self, eng, out, in0, s1, op0, r, w, s2=None, op1=None):
        if op1 is None:
            return self.op(eng, lambda e: e.tensor_scalar(out=out, in0=in0, scalar1=s1, scalar2=None, op0=op0), r=r, w=w)
        return self.op(eng, lambda e: e.tensor_scalar(out=out, in0=in0, scalar1=s1, scalar2=s2, op0=op0, op1=op1), r=r, w=w)

    def stt(self, out, in0, scalar, in1, op0, op1, r, w):
        return self.op("dve", lambda e: e.scalar_tensor_tensor(out=out, in0=in0, scalar=scalar, in1=in1, op0=op0, op1=op1), r=r, w=w)

    def cp(self, eng, out, in_, r, w):
        if eng == "act":
            return self.act(out, in_, AF.Copy, r, w)
        return self.op(eng, lambda e: e.tensor_copy(out=out, in_=in_), r=r, w=w)

    def mm(self, out, lhsT, rhs, start, stop, r, w):
        return self.op("pe", lambda e: e.matmul(out=out, lhsT=lhsT, rhs=rhs, start=start, stop=stop), r=r, w=w)

    def tr(self, out, in_, ident, r, w):
        return self.op("pe", lambda e: e.transpose(out=out, in_=in_, identity=ident), r=r, w=w)

    def memset(self, eng, ap, val, w):
        return self.op(eng, lambda e: e.memset(ap, val), r=(), w=w)

    def recip(self, out, in_, r, w):
        return self.op("dve", lambda e: e.reciprocal(out=out, in_=in_), r=r, w=w)


class Rot:
    def __init__(self, P, name, shape, dt, n, psum=False):
        self.t = [(P.ps if psum else P.sb)(f"{name}{i}", shape, dt) for i in range(n)]
        self.k = [f"{name}{i}" for i in range(n)]
        self.i = -1

    def next(self):
        self.i = (self.i + 1) % len(self.t)
        return self.t[self.i], self.k[self.i]


class Ctx:
    pass


def declare_io(P, layers, debug, single=False):
    g = Ctx()
    g.P = P
    g.layers = layers
    g.debug = debug
    g.single = single
    g.L = (lambda l: 0) if single else (lambda l: l)
    g.J = (lambda j: 0) if single else (lambda j: j)
    nl = 1 if single else 4
    nj = 1 if single else 2
    has_even = any(l % 2 == 0 for l in layers) or not single
    has_odd = any(l % 2 == 1 for l in layers) or not single
    I = lambda n, s: P.dram(n, s, F32, kind="ExternalInput")
    g.xc = I("xc", [NT, D])
    g.cs = I("cs", [128, 16])
    g.w_mod = I("w_mod", [nl, D, 3 * D])
    g.b_mod = I("b_mod", [nl, 3 * D])
    g.g_pre = I("g_pre", [nl, D])
    g.g_post = I("g_post", [nl, D])
    if has_even:
        g.ab_w_in = I("ab_w_in", [nj, D, AB_IN])
        g.ab_b_gate = I("ab_b_gate", [nj, 16])
        g.ab_conv = I("ab_conv", [nj, 128, 48])
        g.ab_mnorm = I("ab_mnorm", [nj, D])
        g.ab_pool_w = I("ab_pool_w", [nj, 4, 256, 256])
        g.ab_pool_scale = I("ab_pool_scale", [nj, 128, 8])
        g.ab_w_out = I("ab_w_out", [nj, 2 * D, D])
        g.k_invcnt = I("k_invcnt", [8, NX + 16])
    if has_odd:
        g.c_w_in = I("c_w_in", [nj, D, C_IN])
        g.c_sink = I("c_sink", [nj, 16])
        g.c_w_out = I("c_w_out", [nj, D, D])
        g.k_rope = I("k_rope", [NT, 64])
    g.k_ident = I("k_ident", [128, 128])
    g.k_trif = I("k_trif", [128, 128])
    g.k_trib = I("k_trib", [128, 128])
    if (not single) or 3 in layers:
        g.out = P.dram("out", [NX, D], F32, kind="ExternalOutput")
    dbg = set(debug) if debug else set()
    S = lambda n, s, dt: P.dram(n, s, dt, kind=("ExternalOutput" if n in dbg else "Internal"))
    g.xcs = [S("xc_a", [NT, D], F32), S("xc_b", [NT, D], F32)]
    g.modv = S("modv", [4, 2, 3, D], F32)
    if has_even:
        g.ut = S("ut", [4096, NT], BF16)
        g.utok = S("utok", [NT, 3072], BF16)
        g.gt = S("gt", [NT, 16], F32)
        g.hf = S("hf", [NT, D], F32)
        g.ypt = S("ypt", [D, NT], BF16)
    if has_odd:
        g.qt = S("qt", [NTILE, 128, 8, 128], BF16)
        g.zt = S("zt", [NT, D], BF16)
    return g


def load_consts(g):
    P = g.P
    g.identf = P.sb("identf", [128, 128], F32)
    g.ident = P.sb("ident", [128, 128], BF16)
    g.trif = P.sb("trif", [128, 128], F32)
    g.trib = P.sb("trib", [128, 128], F32)
    g.trifb = P.sb("trifb", [128, 128], BF16)
    g.tribb = P.sb("tribb", [128, 128], BF16)
    g.onesf = P.sb("onesf", [128, 128], F32)
    g.onesb = P.sb("onesb", [128, 128], BF16)
    g.cc = P.sb("cc", [128, 8], F32)
    P.dma("sp", g.identf[:], g.k_ident, w=["identf"])
    P.dma("sp", g.trif[:], g.k_trif, w=["trif"])
    P.dma("sp", g.trib[:], g.k_trib, w=["trib"])
    P.cp("dve", g.ident[:], g.identf[:], r=["identf"], w=["ident"])
    P.cp("dve", g.trifb[:], g.trif[:], r=["trif"], w=["trifb"])
    P.cp("dve", g.tribb[:], g.trib[:], r=["trib"], w=["tribb"])
    P.memset("pool", g.onesf[:], 1.0, w=["onesf"])
    P.memset("pool", g.onesb[:], 1.0, w=["onesb"])
    P.memset("pool", g.cc[:, 0:1], 0.0, w=["cc"])
    P.memset("pool", g.cc[:, 1:2], 1.0, w=["cc"])
    P.memset("pool", g.cc[:, 2:3], EPS, w=["cc"])
    P.memset("pool", g.cc[:, 3:4], LN16, w=["cc"])
    P.barrier()


def stage_mod(g, l):
    P = g.P
    P.begin_stage()
    wmr = Rot(P, "wm", [128, 3 * D], F32, 2)
    whr = Rot(P, "wh", [128, 3 * D], BF16, 2)
    wlr = Rot(P, "wl", [128, 3 * D], BF16, 2)
    cs = P.sb("cs_sb", [128, 16], F32)
    e1 = P.sb("m_e1", [128, 16], F32)
    sc = P.sb("m_sc", [128, 16], F32)
    sch = P.sb("m_sch", [128, 16], BF16)
    scl = P.sb("m_scl", [128, 16], BF16)
    bm = P.sb("m_bm", [2, 3 * D], F32)
    gp = P.sb("m_gp", [2, 2 * D], F32)
    res = P.sb("m_res", [2, 3 * D], F32)
    o3 = P.sb("m_o3", [2, 3 * D], F32)
    P.dma("sp", cs[:], g.cs, w=["cs"])
    P.dma("sp", bm[:], g.b_mod[g.L(l), :].partition_broadcast(2), w=["bm"])
    P.dma("sp", gp[:, 0:D], g.g_pre[g.L(l), :].partition_broadcast(2), w=["gp"], stream="gp0")
    P.dma("sp", gp[:, D:2 * D], g.g_post[g.L(l), :].partition_broadcast(2), w=["gp"], stream="gp1")
    P.act(e1[:], cs[:], AF.Exp, r=["cs"], w=["e1"], scale=-1.0)
    P.ts("dve", e1[:], e1[:], 1.0, ALU.add, r=["e1"], w=["e1"])
    P.recip(e1[:], e1[:], r=["e1"], w=["e1"])
    P.tt("dve", sc[:], cs[:], e1[:], ALU.mult, r=["cs", "e1"], w=["sc"])
    P.cp("dve", sch[:], sc[:], r=["sc"], w=["sch"])
    P.tt("dve", scl[:], sc[:], sch[:], ALU.subtract, r=["sc", "sch"], w=["scl"])
    pm = [P.ps(f"m_ps{i}", [2, 512], F32) for i in range(6)]
    for k in range(8):
        wm, wmk = wmr.next()
        wh, whk = whr.next()
        wl, wlk = wlr.next()
        P.dma("sp", wm[:], g.w_mod[g.L(l), k * 128:(k + 1) * 128, :], w=[wmk])
        P.cp("act", wh[:], wm[:], r=[wmk], w=[whk])
        P.tt("dve", wl[:], wm[:], wh[:], ALU.subtract, r=[wmk, whk], w=[wlk])
        for n in range(6):
            cs_ = slice(n * 512, (n + 1) * 512)
            P.mm(pm[n][:], sch[:, 2 * k:2 * k + 2], wh[:, cs_], k == 0, False, r=["sch", whk], w=[f"mps{n}"])
            P.mm(pm[n][:], scl[:, 2 * k:2 * k + 2], wh[:, cs_], False, False, r=["scl", whk], w=[f"mps{n}"])
            P.mm(pm[n][:], sch[:, 2 * k:2 * k + 2], wl[:, cs_], False, k == 7, r=["sch", wlk], w=[f"mps{n}"])
    for n in range(6):
        P.tt("dve", res[:, n * 512:(n + 1) * 512], pm[n][:], bm[:, n * 512:(n + 1) * 512], ALU.add,
             r=[f"mps{n}", "bm"], w=["res"])
    P.stt(o3[:, 0:D], res[:, D:2 * D], 1.0, gp[:, 0:D], ALU.add, ALU.mult, r=["res", "gp"], w=["o3"])
    P.cp("dve", o3[:, D:2 * D], res[:, 0:D], r=["res"], w=["o3"])
    P.tt("dve", o3[:, 2 * D:3 * D], res[:, 2 * D:3 * D], gp[:, D:2 * D], ALU.mult, r=["res", "gp"], w=["o3"])
    P.dma("sp", g.modv[l].rearrange("s a d -> s (a d)"), o3[:], r=["o3"], w=["d:modv"])
    P.end_stage()


def norm_mod_tile(g, pf, xt, xk, A, B, src, cc, hb, hbk, junk, stat):
    P = g.P
    P.act(junk[:], xt[:], AF.Square, r=[xk], w=[pf + "junk", pf + "ss"], accum=stat[:, 0:1])
    P.act(stat[:, 1:2], stat[:, 0:1], AF.Ln, r=[pf + "ss"], w=[pf + "ln"], bias=cc[:, 2:3], scale=1.0 / D)
    P.act(stat[:, 2:3], stat[:, 1:2], AF.Exp, r=[pf + "ln"], w=[pf + "rstd"], scale=-0.5)
    P.stt(junk[:], xt[:], stat[:, 2:3], A[:, src, :], ALU.mult, ALU.mult, r=[xk, pf + "rstd", "AB"], w=[pf + "junk"])
    P.tt("pool", hb[:], junk[:], B[:, src, :], ALU.add, r=[pf + "junk", "AB"], w=[hbk])


def load_AB(g, l, A, B):
    P = g.P
    for s in range(2):
        P.dma("sp", A[:, s, :], g.modv[l, s, 0, :].partition_broadcast(128), w=["AB"], stream=f"ab{s}0")
        P.dma("sp", B[:, s, :], g.modv[l, s, 1, :].partition_broadcast(128), w=["AB"], stream=f"ab{s}1")


def blocks():
    return [(0, 2)] + [(2 + 4 * i, 4) for i in range(8)]


def stage_even_A(g, l, xin):
    P = g.P
    j = l // 2
    P.begin_stage()
    w = P.sb("wA", [128, 8, AB_IN], BF16)
    for k in range(8):
        P.dma("pool", w[:, k, :], g.ab_w_in[g.J(j), k * 128:(k + 1) * 128, :], w=[f"wA{k}"])
    wkeys = [f"wA{k}" for k in range(8)]
    A = P.sb("A_A", [128, 2, D], F32)
    B = P.sb("A_B", [128, 2, D], F32)
    load_AB(g, l, A, B)
    xr = Rot(P, "A_x", [128, D], F32, 2)
    junk = P.sb("A_junk", [128, D], F32)
    stat = Rot(P, "A_stat", [128, 4], F32, 2)
    hbr = Rot(P, "A_hb", [128, D], BF16, 2)
    hT = P.sb("A_hT", [128, 8, 512], BF16)
    ptr = Rot(P, "A_ptr", [128, D], BF16, 1, psum=True)
    pu = Rot(P, "A_pu", [128, 512], F32, 6, psum=True)
    utr = Rot(P, "A_ut", [128, 3072], BF16, 2)
    gtr = Rot(P, "A_gt", [128, 16], F32, 2)
    uTr = Rot(P, "A_uT", [128, 512], BF16, 3)
    tog = 0
    for (t0, nt) in blocks():
        P.split()
        src = 1 if t0 == 0 else 0
        TB = nt * 128
        for ti in range(nt):
            tile = t0 + ti
            xt, xk = xr.next()
            st, sk = stat.next()
            hb, hbk = hbr.next()
            P.dma("sp", xt[:], xin[tile * 128:(tile + 1) * 128, :], r=["d:xin"], w=[xk])
            norm_mod_tile(g, sk, xt, xk, A, B, src, g.cc, hb, hbk, junk, st)
            pt, ptk = ptr.next()
            for k in range(8):
                P.tr(pt[:, k * 128:(k + 1) * 128], hb[:, k * 128:(k + 1) * 128], g.ident[:], r=[hbk, "ident"], w=[ptk])
            P.cp("act", hT[:, :, ti * 128:(ti + 1) * 128], pt[:].rearrange("p (k t) -> p k t", k=8), r=[ptk], w=[f"hT{ti}"])
            ut, utk = utr.next()
            for n in range(6):
                ps, psk = pu.next()
                c0 = 2048 + n * 512
                for k in range(8):
                    P.mm(ps[:], hT[:, k, ti * 128:(ti + 1) * 128], w[:, k, c0:c0 + 512], k == 0, k == 7,
                         r=[f"hT{ti}", wkeys[k]], w=[psk])
                eng = "act" if (tog % 2 == 0) else "dve"
                tog += 1
                P.cp(eng, ut[:, n * 512:(n + 1) * 512], ps[:], r=[psk], w=[utk])
            P.dma("pool", g.utok[tile * 128:(tile + 1) * 128, :], ut[:], r=[utk], w=["d:utok"])
            ps, psk = pu.next()
            gtt, gtk = gtr.next()
            for k in range(8):
                P.mm(ps[:, 0:16], hT[:, k, ti * 128:(ti + 1) * 128], w[:, k, GATE_OFF:GATE_OFF + 16], k == 0, k == 7,
                     r=[f"hT{ti}", wkeys[k]], w=[psk])
            P.cp("dve", gtt[:], ps[:, 0:16], r=[psk], w=[gtk])
            P.dma("pool", g.gt[tile * 128:(tile + 1) * 128, :], gtt[:], r=[gtk], w=["d:gt"])
        hkeys = [f"hT{ti}" for ti in range(nt)]
        for n in range(32):
            c0 = n * 128 if n < 16 else 5120 + (n - 16) * 128
            ps, psk = pu.next()
            for k in range(8):
                P.mm(ps[:, 0:TB], w[:, k, c0:c0 + 128], hT[:, k, 0:TB], k == 0, k == 7, r=hkeys + [wkeys[k]], w=[psk])
            uT, uTk = uTr.next()
            eng = "act" if (tog % 2 == 0) else "dve"
            tog += 1
            P.cp(eng, uT[:, 0:TB], ps[:, 0:TB], r=[psk], w=[uTk])
            P.dma("pool", g.ut[n * 128:(n + 1) * 128, t0 * 128:t0 * 128 + TB], uT[:, 0:TB], r=[uTk], w=["d:ut"])
    P.end_stage()


def stage_even_P(g, l):
    P = g.P
    j = l // 2
    P.begin_stage()
    pw = P.sb("P_pw", [128, 8, 256], BF16)
    pwv = g.ab_pool_w[g.J(j)].rearrange("g (cj p) d -> p (g cj) d", p=128)
    for i8 in range(8):
        P.dma("pool", pw[:, i8, :], pwv[:, i8, :], w=["pw"])
    psc = P.sb("P_psc", [128, 8], F32)
    P.dma("sp", psc[:], g.ab_pool_scale[g.J(j)], w=["psc"])
    PT = P.sb("P_PT", [128, 8, NT], BF16)
    inv = P.sb("P_inv", [128, NX + 16], F32)
    raw = Rot(P, "P_raw", [128, NX + 16], BF16, 2)
    f1 = P.sb("P_f1", [128, NX + 16], F32)
    f2 = P.sb("P_f2", [128, NX + 16], F32)
    for gi in range(4):
        for (n, tok0, invrow) in ((NX, NCTX, gi), (NCTX, 0, 4 + gi)):
            P.dma("sp", inv[:, 0:n + 16], g.k_invcnt[invrow, 0:n + 16].partition_broadcast(128), w=["inv"])
            for cj in range(2):
                ct = 2 * gi + cj
                rw, rwk = raw.next()
                P.memset("pool", rw[:, 0:8], 0.0, w=[rwk])
                P.memset("pool", rw[:, 8 + n:16 + n], 0.0, w=[rwk])
                P.dma("sp", rw[:, 8:8 + n], g.ut[2048 + ct * 128:2048 + (ct + 1) * 128, tok0:tok0 + n], r=["d:ut"], w=[rwk])
                P.tt("dve", f1[:, 1:15 + n], rw[:, 0:14 + n], rw[:, 1:15 + n], ALU.add, r=[rwk], w=["f1"])
                res, resk = f1, "f1"
                if gi >= 1:
                    P.tt("pool", f2[:, 2:14 + n], f1[:, 1:13 + n], f1[:, 3:15 + n], ALU.add, r=["f1"], w=["f2"])
                    res, resk = f2, "f2"
                if gi >= 2:
                    P.tt("dve", f1[:, 4:12 + n], f2[:, 2:10 + n], f2[:, 6:14 + n], ALU.add, r=["f2"], w=["f1"])
                    res, resk = f1, "f1"
                if gi >= 3:
                    P.tt("pool", f2[:, 8:8 + n], f1[:, 4:4 + n], f1[:, 12:12 + n], ALU.add, r=["f1"], w=["f2"])
                    res, resk = f2, "f2"
                P.tt("dve", res[:, 8:8 + n], res[:, 8:8 + n], inv[:, 8:8 + n], ALU.mult, r=[resk, "inv"], w=[resk])
                P.tt("pool", PT[:, ct, tok0:tok0 + n], res[:, 8:8 + n], rw[:, 8:8 + n], ALU.subtract, r=[resk, rwk], w=[f"PT{ct}"])
    pps = Rot(P, "P_ps", [128, 512], F32, 2, psum=True)
    zr = Rot(P, "P_z", [128, 512], BF16, 2)
    er = Rot(P, "P_e", [128, 512], F32, 2)
    yr = Rot(P, "P_y", [128, 512], BF16, 2)
    for (t0, nt) in blocks():
        P.split()
        TB = nt * 128
        tok0 = t0 * 128
        for gi in range(4):
            for dt in range(2):
                idx = 2 * gi + dt
                ps, psk = pps.next()
                for cj in range(2):
                    P.mm(ps[:, 0:TB], pw[:, gi * 2 + cj, dt * 128:(dt + 1) * 128], PT[:, 2 * gi + cj, tok0:tok0 + TB], cj == 0, cj == 1,
                         r=["pw", f"PT{2 * gi + cj}"], w=[psk])
                z, zk = zr.next()
                e, ek = er.next()
                y, yk = yr.next()
                P.dma("sp", z[:, 0:TB], g.ut[3072 + idx * 128:3072 + (idx + 1) * 128, tok0:tok0 + TB], r=["d:ut"], w=[zk])
                P.act(e[:, 0:TB], z[:, 0:TB], AF.Exp, r=[zk], w=[ek], scale=-1.0)
                P.ts("pool", e[:, 0:TB], e[:, 0:TB], 1.0, ALU.add, r=[ek], w=[ek])
                P.recip(e[:, 0:TB], e[:, 0:TB], r=[ek], w=[ek])
                P.tt("pool", e[:, 0:TB], e[:, 0:TB], z[:, 0:TB], ALU.mult, r=[ek, zk], w=[ek])
                P.stt(y[:, 0:TB], ps[:, 0:TB], psc[:, idx:idx + 1], e[:, 0:TB], ALU.mult, ALU.mult, r=[psk, "psc", ek], w=[yk])
                P.dma("pool", g.ypt[idx * 128:(idx + 1) * 128, tok0:tok0 + TB], y[:, 0:TB], r=[yk], w=["d:ypt"])
    P.end_stage()


def stage_even_S(g, l, xin, xout, last):
    P = g.P
    j = l // 2
    P.begin_stage()
    wo = P.sb("S_wo", [128, 16, D], BF16)
    for k in range(16):
        P.dma("pool", wo[:, k, :], g.ab_w_out[g.J(j), k * 128:(k + 1) * 128, :], w=[f"wo{k}"])
    cw = P.sb("S_cw", [128, 48], F32)
    P.dma("sp", cw[:], g.ab_conv[g.J(j)], w=["cw"])
    bg = P.sb("S_bg", [128, 16], F32)
    P.dma("sp", bg[:], g.ab_b_gate[g.J(j), :].partition_broadcast(128), w=["bg"])
    mn = P.sb("S_mn", [128, D], F32)
    P.dma("sp", mn[:], g.ab_mnorm[g.J(j), :].partition_broadcast(128), w=["mn"])
    GG = P.sb("S_GG", [128, 2, D], F32)
    for s in range(2):
        P.dma("sp", GG[:, s, :], g.modv[l, s, 2, :].partition_broadcast(128), w=["GG"], stream=f"gg{s}")
    Wb = [[P.sb(f"S_Wb{a}{t}", [128, 8, 128], F32) for t in range(3)] for a in range(2)]
    cwv = cw[:].rearrange("p (c t) -> p c t", t=3)
    for a in range(2):
        for t in range(3):
            P.cp("dve", Wb[a][t][:], cwv[:, a * 8:(a + 1) * 8, t:t + 1].to_broadcast([128, 8, 128]), r=["cw"], w=[f"Wb{a}{t}"])
    C = P.sb("S_C", [128, 4, 2, 257], F32)
    Cb = P.sb("S_Cb", [128, 4, 2, 257], BF16)
    raws = [Rot(P, "S_qr", [128, 8, 130], BF16, 2), Rot(P, "S_kr", [128, 8, 130], BF16, 2)]
    cvt = [P.sb(f"S_cvt{i}", [128, 8, 128], F32) for i in range(3)]
    qkc = [Rot(P, "S_qc", [128, 8, 128], BF16, 2), Rot(P, "S_kc", [128, 8, 128], BF16, 2)]
    ktokr = Rot(P, "S_ktok", [128, D], BF16, 2)
    vtokr = Rot(P, "S_vtok", [128, 3072], BF16, 2)
    gtr = Rot(P, "S_gt", [128, 16], F32, 2)
    gw = Rot(P, "S_gw", [128, 48], F32, 2)
    lhr = Rot(P, "S_lh", [128, 8], BF16, 2)
    v1r = Rot(P, "S_v1", [128, 4, 257], BF16, 2)
    v2r = Rot(P, "S_v2", [128, 4, 257], BF16, 2)
    smr = Rot(P, "S_sm", [128, 128], BF16, 2)
    dnr = Rot(P, "S_dn", [128, 8], F32, 2)
    hhr = Rot(P, "S_hh", [128, D], F32, 2)
    hfr = Rot(P, "S_hf", [128, D], F32, 2)
    xr = Rot(P, "S_x", [128, D], F32, 2)
    ypr = Rot(P, "S_yp", [128, 8, 128], BF16, 2)
    t1 = P.sb("S_t1", [128, D], F32)
    t2 = P.sb("S_t2", [128, D], F32)
    t3 = P.sb("S_t3", [128, D], F32)
    st4 = Rot(P, "S_st4", [128, 16], F32, 2)
    ymr = Rot(P, "S_ym", [128, D], BF16, 2)
    ymTr = Rot(P, "S_ymT", [128, 8, 128], BF16, 2)
    xor_ = Rot(P, "S_xo", [128, D], F32, 2)
    p_tr = Rot(P, "S_ptr", [128, D], BF16, 1, psum=True)
    p_g = Rot(P, "S_pg", [128, 8], F32, 1, psum=True)
    p_s = Rot(P, "S_pst", [128, 128], F32, 1, psum=True)
    p_in = Rot(P, "S_pin", [128, 257], F32, 1, psum=True)
    p_c = [Rot(P, "S_pc0", [128, 257], F32, 1, psum=True), Rot(P, "S_pc1", [128, 257], F32, 1, psum=True)]
    p_wo = [Rot(P, "S_pw0", [128, 512], F32, 1, psum=True), Rot(P, "S_pw1", [128, 512], F32, 1, psum=True)]
    utq = [g.ut[0:1024, :].rearrange("(j p) t -> p j t", p=128), g.ut[1024:2048, :].rearrange("(j p) t -> p j t", p=128)]
    yptv = g.ypt.rearrange("(j p) t -> p j t", p=128)
    wokeys = [f"wo{k}" for k in range(16)]

    import os
    _dirs = tuple(int(c) for c in os.environ.get("KDIRS", "01"))
    _ntl = int(os.environ.get("KNT", "99"))
    _off = os.environ.get("KOFF", "")
    for dirn in _dirs:
        P.memset("pool", C[:], 0.0, w=["C"])
        P.memset("pool", Cb[:], 0.0, w=["Cb"])
        order = list(range(NTILE)) if dirn == 0 else [1, 0] + list(range(NTILE - 1, 1, -1))
        tri = g.trif if dirn == 0 else g.trib
        trib16 = g.trifb if dirn == 0 else g.tribb
        trik = "trif" if dirn == 0 else "trib"
        for tile in order[:_ntl]:
            P.split()
            isctx = tile < 2
            src = 1 if isctx else 0
            need_out = not (last and isctx)
            seq_lo = tile in (0, 2)
            seq_hi = tile in (1, NTILE - 1)
            tok = tile * 128
            c_lo = 1 if seq_lo else 0
            c_hi = 129 if seq_hi else 130
            conv = []
            for a in range(2):
                rw, rwk = raws[a].next()
                if seq_lo:
                    P.memset("pool", rw[:, :, 0:1], 0.0, w=[rwk])
                if seq_hi:
                    P.memset("pool", rw[:, :, 129:130], 0.0, w=[rwk])
                for ct in range(8):
                    P.dma("sp", rw[:, ct, c_lo:c_hi], utq[a][:, ct, tok - 1 + c_lo:tok - 1 + c_hi], r=["d:ut"], w=[rwk])
                oc, ock = qkc[a].next()
                if a == 1 or need_out:
                    P.tt("pool", cvt[0][:], rw[:, :, 0:128], Wb[a][0][:], ALU.mult, r=[rwk, f"Wb{a}0"], w=["cvt0"])
                    P.tt("dve", cvt[1][:], rw[:, :, 1:129], Wb[a][1][:], ALU.mult, r=[rwk, f"Wb{a}1"], w=["cvt1"])
                    P.tt("pool", cvt[2][:], rw[:, :, 2:130], Wb[a][2][:], ALU.mult, r=[rwk, f"Wb{a}2"], w=["cvt2"])
                    P.tt("dve", cvt[0][:], cvt[0][:], cvt[1][:], ALU.add, r=["cvt0", "cvt1"], w=["cvt0"])
                    P.tt("pool", oc[:], cvt[0][:], cvt[2][:], ALU.add, r=["cvt0", "cvt2"], w=[ock])
                conv.append((oc, ock))
            (qc, qck), (kc, kck) = conv
            pt, ptk = p_tr.next()
            for ct in range(8):
                P.tr(pt[:, ct * 128:(ct + 1) * 128], kc[:, ct, :], g.ident[:], r=[kck, "ident"], w=[ptk])
            ktok, ktk = ktokr.next()
            P.cp("act", ktok[:], pt[:], r=[ptk], w=[ktk])
            vtok, vtk = vtokr.next()
            P.dma("sp", vtok[:], g.utok[tok:tok + 128, :], r=["d:utok"], w=[vtk])
            gtt, gtk = gtr.next()
            P.dma("sp", gtt[:], g.gt[tok:tok + 128, :], r=["d:gt"], w=[gtk])
            G, Gk = gw.next()
            gg = G[:, 0:16]
            l1, tmp, tmp2, ea, ea2, eb, edec, e1 = [G[:, 16 + 4 * i:20 + 4 * i] for i in range(8)]
            P.tt("pool", gg, gtt[:], bg[:], ALU.add, r=[gtk, "bg"], w=[Gk + "g"])
            li = G[:, 8 * dirn:8 * dirn + 4]
            fr = G[:, 8 * dirn + 4:8 * dirn + 8]
            P.act(e1, fr, AF.Exp, r=[Gk + "g"], w=[Gk + "e1"], scale=-1.0)
            P.act(l1, e1, AF.Ln, r=[Gk + "e1"], w=[Gk + "l1"], bias=g.cc[:, 1:2], scale=1.0)
            pg, pgk = p_g.next()
            lh, lhk = lhr.next()
            P.cp("dve", lh[:, 0:4], l1, r=[Gk + "l1"], w=[lhk])
            P.tt("dve", lh[:, 4:8], l1, lh[:, 0:4], ALU.subtract, r=[Gk + "l1", lhk], w=[lhk])
            P.mm(pg[:, 0:4], trib16[:], lh[:, 0:4], True, False, r=[trik + "b", lhk], w=[pgk])
            P.mm(pg[:, 0:4], trib16[:], lh[:, 4:8], False, True, r=[trik + "b", lhk], w=[pgk])
            P.mm(pg[:, 4:8], g.onesb[:], lh[:, 0:4], True, False, r=["onesb", lhk], w=[pgk])
            P.mm(pg[:, 4:8], g.onesb[:], lh[:, 4:8], False, True, r=["onesb", lhk], w=[pgk])
            P.tt("dve", tmp, li, pg[:, 0:4], ALU.add, r=[Gk + "g", pgk], w=[Gk + "tmp"])
            P.tt("dve", tmp2, tmp, pg[:, 4:8], ALU.subtract, r=[Gk + "tmp", pgk], w=[Gk + "tmp2"])
            P.act(ea, tmp, AF.Exp, r=[Gk + "tmp"], w=[Gk + "ea"], bias=g.cc[:, 3:4], scale=1.0)
            P.act(ea2, tmp2, AF.Exp, r=[Gk + "tmp2"], w=[Gk + "ea2"], bias=g.cc[:, 3:4], scale=1.0)
            P.act(eb, pg[:, 0:4], AF.Exp, r=[pgk], w=[Gk + "eb"], scale=-1.0)
            P.act(edec, pg[:, 4:8], AF.Exp, r=[pgk], w=[Gk + "edec"], scale=-1.0)
            v1, v1k = v1r.next()
            v2, v2k = v2r.next()
            for h in range(4):
                if need_out:
                    P.ts("dve", v1[:, h, 0:256], vtok[:, h * 256:(h + 1) * 256], ea[:, h:h + 1], ALU.mult, r=[vtk, Gk + "ea"], w=[v1k])
                P.ts("pool", v2[:, h, 0:256], vtok[:, h * 256:(h + 1) * 256], ea2[:, h:h + 1], ALU.mult, r=[vtk, Gk + "ea2"], w=[v2k])
            if need_out:
                P.cp("dve", v1[:, :, 256], ea, r=[Gk + "ea"], w=[v1k])
            P.cp("pool", v2[:, :, 256], ea2, r=[Gk + "ea2"], w=[v2k])
            hh, hhk = hhr.next()
            for h in range(4):
                if need_out:
                    ps_, psk_ = p_s.next()
                    for jj in range(2):
                        P.mm(ps_[:], kc[:, 2 * h + jj, :], qc[:, 2 * h + jj, :], jj == 0, jj == 1, r=[kck, qck], w=[psk_])
                    sm, smk = smr.next()
                    P.tt("dve", sm[:], ps_[:], tri[:], ALU.mult, r=[psk_, trik], w=[smk])
                    pi, pik = p_in.next()
                    for jj in range(2):
                        P.mm(pi[:], qc[:, 2 * h + jj, :], Cb[:, h, jj, :], jj == 0, False, r=[qck, "Cb"], w=[pik])
                    P.mm(pi[:], sm[:], v1[:, h, :], False, True, r=[smk, v1k], w=[pik])
                    dn, dnk = dnr.next()
                    P.ts("dve", dn[:, 0:1], pi[:, 256:257], eb[:, h:h + 1], ALU.mult, r=[pik, Gk + "eb"], w=[dnk])
                    P.stt(dn[:, 1:2], dn[:, 0:1], -1.0, dn[:, 0:1], ALU.mult, ALU.max, r=[dnk], w=[dnk])
                    P.ts("dve", dn[:, 2:3], dn[:, 1:2], 1.0, ALU.max, r=[dnk], w=[dnk])
                    P.recip(dn[:, 3:4], dn[:, 2:3], r=[dnk], w=[dnk])
                    P.tt("dve", dn[:, 4:5], dn[:, 3:4], eb[:, h:h + 1], ALU.mult, r=[dnk, Gk + "eb"], w=[dnk])
                    P.ts("dve", hh[:, h * 256:(h + 1) * 256], pi[:, 0:256], dn[:, 4:5], ALU.mult, r=[pik, dnk], w=[hhk])
                for jj in range(2):
                    pc, pck = p_c[jj].next()
                    P.mm(pc[:], ktok[:, h * 256 + jj * 128:h * 256 + (jj + 1) * 128], v2[:, h, :], True, True, r=[ktk, v2k], w=[pck])
                    P.stt(C[:, h, jj, :], C[:, h, jj, :], edec[:, h:h + 1], pc[:], ALU.mult, ALU.add, r=["C", Gk + "edec", pck], w=["C"])
                    P.cp("pool", Cb[:, h, jj, :], C[:, h, jj, :], r=["C"], w=["Cb"])
            if not need_out:
                continue
            if dirn == 0:
                P.dma("pool", g.hf[tok:tok + 128, :], hh[:], r=[hhk], w=["d:hf"])
                continue
            hf, hfk = hfr.next()
            P.dma("sp", hf[:], g.hf[tok:tok + 128, :], r=["d:hf"], w=[hfk])
            xt, xk = xr.next()
            P.dma("sp", xt[:], xin[tok:tok + 128, :], r=["d:xin"], w=[xk])
            yp, ypk = ypr.next()
            for ct in range(8):
                P.dma("sp", yp[:, ct, :], yptv[:, ct, tok:tok + 128], r=["d:ypt"], w=[ypk])
            P.tt("pool", hh[:], hh[:], hf[:], ALU.add, r=[hhk, hfk], w=[hhk])
            P.act(t1[:], vtok[:, 1024:2048], AF.Exp, r=[vtk], w=["t1"], scale=-1.0)
            P.ts("pool", t1[:], t1[:], 1.0, ALU.add, r=["t1"], w=["t1"])
            P.recip(t1[:], t1[:], r=["t1"], w=["t1"])
            P.tt("pool", hh[:], hh[:], t1[:], ALU.mult, r=[hhk, "t1"], w=[hhk])
            s4, s4k = st4.next()
            for h in range(4):
                P.act(t1[:, h * 256:(h + 1) * 256], hh[:, h * 256:(h + 1) * 256], AF.Square, r=[hhk], w=["t1", s4k],
                      accum=s4[:, h:h + 1])
            P.act(s4[:, 4:8], s4[:, 0:4], AF.Ln, r=[s4k], w=[s4k], bias=g.cc[:, 2:3], scale=1.0 / 256.0)
            P.act(s4[:, 8:12], s4[:, 4:8], AF.Exp, r=[s4k], w=[s4k], scale=-0.5)
            P.act(t2[:], vtok[:, 2048:3072], AF.Exp, r=[vtk], w=["t2"], scale=-1.0)
            P.ts("pool", t2[:], t2[:], 1.0, ALU.add, r=["t2"], w=["t2"])
            P.recip(t2[:], t2[:], r=["t2"], w=["t2"])
            P.tt("pool", t2[:], t2[:], vtok[:, 2048:3072], ALU.mult, r=["t2", vtk], w=["t2"])
            P.tt("pool", t2[:], t2[:], mn[:], ALU.mult, r=["t2", "mn"], w=["t2"])
            ym, ymk = ymr.next()
            for h in range(4):
                P.stt(ym[:, h * 256:(h + 1) * 256], hh[:, h * 256:(h + 1) * 256], s4[:, 8 + h:9 + h], t2[:, h * 256:(h + 1) * 256],
                      ALU.mult, ALU.mult, r=[hhk, s4k, "t2"], w=[ymk])
            pt, ptk = p_tr.next()
            for ct in range(8):
                P.tr(pt[:, ct * 128:(ct + 1) * 128], ym[:, ct * 128:(ct + 1) * 128], g.ident[:], r=[ymk, "ident"], w=[ptk])
            ymT, ymTk = ymTr.next()
            P.cp("act", ymT[:], pt[:].rearrange("p (k t) -> p k t", k=8), r=[ptk], w=[ymTk])
            pws = []
            for nt_ in range(2):
                pw_, pwk_ = p_wo[nt_].next()
                for f in range(16):
                    lhs = ymT[:, f, :] if f < 8 else yp[:, f - 8, :]
                    lk = ymTk if f < 8 else ypk
                    P.mm(pw_[:], lhs, wo[:, f, nt_ * 512:(nt_ + 1) * 512], f == 0, f == 15, r=[lk, wokeys[f]], w=[pwk_])
                pws.append((pw_, pwk_))
            post_norm_residual(g, pws, xt, xk, GG, src, t3, st4, xor_, xout, tile, last)
    P.end_stage()


def post_norm_residual(g, pws, xt, xk, GG, src, t3, st4, xor_, xout, tile, last):
    P = g.P
    s5, s5k = st4.next()
    for nt_ in range(2):
        pw_, pwk_ = pws[nt_]
        P.act(t3[:, nt_ * 512:(nt_ + 1) * 512], pw_[:], AF.Square, r=[pwk_], w=["t3", s5k], accum=s5[:, nt_:nt_ + 1])
    P.tt("dve", s5[:, 2:3], s5[:, 0:1], s5[:, 1:2], ALU.add, r=[s5k], w=[s5k])
    P.act(s5[:, 3:4], s5[:, 2:3], AF.Ln, r=[s5k], w=[s5k], bias=g.cc[:, 2:3], scale=1.0 / D)
    P.act(s5[:, 4:5], s5[:, 3:4], AF.Exp, r=[s5k], w=[s5k], scale=-0.5)
    xo, xok = xor_.next()
    for nt_ in range(2):
        pw_, pwk_ = pws[nt_]
        P.stt(t3[:, nt_ * 512:(nt_ + 1) * 512], pw_[:], s5[:, 4:5], GG[:, src, nt_ * 512:(nt_ + 1) * 512], ALU.mult, ALU.mult,
              r=[pwk_, s5k, "GG"], w=["t3"])
    P.tt("pool", xo[:], t3[:], xt[:], ALU.add, r=["t3", xk], w=[xok])
    tok = tile * 128
    if last:
        P.dma("pool", g.out[tok - NCTX:tok - NCTX + 128, :], xo[:], r=[xok], w=["d:xout"])
    else:
        P.dma("pool", xout[tok:tok + 128, :], xo[:], r=[xok], w=["d:xout"])


def stage_odd(g, l, xin, xout, last):
    P = g.P
    j = l // 2
    P.begin_stage()
    kT = P.sb("O_kT", [128, 4, NT], BF16)
    va = P.sb("O_va", [128, NTILE, 4, 65], BF16)
    P.memset("pool", va[:, :, :, 64:65], 1.0, w=["va_ones"])
    P.begin_stage()
    w = P.sb("OA_w", [128, 8, C_IN], BF16)
    for k in range(8):
        P.dma("pool", w[:, k, :], g.c_w_in[g.J(j), k * 128:(k + 1) * 128, :], w=[f"wC{k}"])
    wkeys = [f"wC{k}" for k in range(8)]
    A = P.sb("OA_A", [128, 2, D], F32)
    B = P.sb("OA_B", [128, 2, D], F32)
    load_AB(g, l, A, B)
    xr = Rot(P, "OA_x", [128, D], F32, 2)
    junk = P.sb("OA_junk", [128, D], F32)
    stat = Rot(P, "OA_stat", [128, 4], F32, 2)
    hbr = Rot(P, "OA_hb", [128, D], BF16, 2)
    hTr = Rot(P, "OA_hT", [128, 8, 128], BF16, 2)
    rpr = Rot(P, "OA_rp", [128, 64], F32, 2)
    tmr = [P.sb(f"OA_tm{i}", [128, 8, 32], F32) for i in range(4)]
    qrr = Rot(P, "OA_qr", [128, 16, 64], BF16, 2)
    kdr = Rot(P, "OA_kd", [128, 4, 2, 64], BF16, 2)
    qTr = Rot(P, "OA_qT", [128, 8, 128], BF16, 2)
    zbr = Rot(P, "OA_zb", [128, D], BF16, 2)
    p_h = Rot(P, "OA_ph", [128, D], BF16, 1, psum=True)
    p_u = Rot(P, "OA_pu", [128, 512], F32, 3, psum=True)
    p_q = Rot(P, "OA_pq", [128, D], BF16, 1, psum=True)
    p_k = Rot(P, "OA_pk", [128, 512], BF16, 1, psum=True)
    for tile in range(NTILE):
        P.split()
        src = 1 if tile < 2 else 0
        tok = tile * 128
        xt, xk = xr.next()
        st, sk = stat.next()
        hb, hbk = hbr.next()
        P.dma("sp", xt[:], xin[tok:tok + 128, :], r=["d:xin"], w=[xk])
        rp, rpk = rpr.next()
        P.dma("sp", rp[:], g.k_rope[tok:tok + 128, :], w=[rpk])
        norm_mod_tile(g, sk, xt, xk, A, B, src, g.cc, hb, hbk, junk, st)
        import os
        _ka = int(os.environ.get("KA", "9"))
        if _ka <= 1:
            continue
        pt, ptk = p_h.next()
        for k in range(8):
            P.tr(pt[:, k * 128:(k + 1) * 128], hb[:, k * 128:(k + 1) * 128], g.ident[:], r=[hbk, "ident"], w=[ptk])
        hT, hTk = hTr.next()
        P.cp("act", hT[:], pt[:].rearrange("p (k t) -> p k t", k=8), r=[ptk], w=[hTk])
        qr, qrk = qrr.next()
        kd, kdk = kdr.next()
        zb, zbk = zbr.next()
        for n in range(5):
            ps, psk = p_u.next()
            for k in range(8):
                P.mm(ps[:], hT[:, k, :], w[:, k, n * 512:(n + 1) * 512], k == 0, k == 7, r=[hTk, wkeys[k]], w=[psk])
            if n < 2 or n == 2:
                nh = 8 if n < 2 else 4
                pv = ps[:, 0:nh * 64].rearrange("p (h i two) -> p h i two", h=nh, two=2)
                x1 = pv[:, :, :, 0]
                x2 = pv[:, :, :, 1]
                cosb = rp[:, 0:32].unsqueeze(1).to_broadcast([128, nh, 32])
                sinb = rp[:, 32:64].unsqueeze(1).to_broadcast([128, nh, 32])
                tm = [t[:, 0:nh, :] for t in tmr]
                P.tt("dve", tm[0], x1, cosb, ALU.mult, r=[psk, rpk], w=["tm0"])
                P.tt("dve", tm[1], x2, sinb, ALU.mult, r=[psk, rpk], w=["tm1"])
                P.tt("dve", tm[2], x1, sinb, ALU.mult, r=[psk, rpk], w=["tm2"])
                P.tt("dve", tm[3], x2, cosb, ALU.mult, r=[psk, rpk], w=["tm3"])
                if n < 2:
                    P.tt("pool", qr[:, 8 * n:8 * n + 8, 0:32], tm[0], tm[1], ALU.subtract, r=["tm0", "tm1"], w=[qrk])
                    P.tt("pool", qr[:, 8 * n:8 * n + 8, 32:64], tm[2], tm[3], ALU.add, r=["tm2", "tm3"], w=[qrk])
                else:
                    for c in range(2):
                        P.tt("pool", kd[:, :, c, 0:32], tm[0], tm[1], ALU.subtract, r=["tm0", "tm1"], w=[kdk])
                        P.tt("pool", kd[:, :, c, 32:64], tm[2], tm[3], ALU.add, r=["tm2", "tm3"], w=[kdk])
                    P.cp("act", va[:, tile, :, 0:64], ps[:, 256:512].rearrange("p (g d) -> p g d", g=4), r=[psk], w=[f"va{tile}"])
            else:
                P.cp("act", zb[:, (n - 3) * 512:(n - 2) * 512], ps[:], r=[psk], w=[zbk])
        if _ka <= 3:
            continue
        P.dma("pool", g.zt[tok:tok + 128, :], zb[:], r=[zbk], w=["d:zt"])
        if _ka <= 4:
            continue
        pq, pqk = p_q.next()
        qrf = qr[:].rearrange("p h d -> p (h d)")
        for k in range(8):
            P.tr(pq[:, k * 128:(k + 1) * 128], qrf[:, k * 128:(k + 1) * 128], g.ident[:], r=[qrk, "ident"], w=[pqk])
        qT, qTk = qTr.next()
        P.cp("dve", qT[:], pq[:].rearrange("p (k t) -> p k t", k=8), r=[pqk], w=[qTk])
        P.dma("pool", g.qt[tile].rearrange("p j t -> p (j t)"), qT[:].rearrange("p j t -> p (j t)"), r=[qTk], w=["d:qt"])
        pk, pkk = p_k.next()
        kdf = kd[:].rearrange("p g c d -> p (g c d)")
        for gq in range(4):
            P.tr(pk[:, gq * 128:(gq + 1) * 128], kdf[:, gq * 128:(gq + 1) * 128], g.ident[:], r=[kdk, "ident"], w=[pkk])
        P.cp("act", kT[:, :, tok:tok + 128], pk[:].rearrange("p (g t) -> p g t", g=4), r=[pkk], w=[f"kT{tile}"])
    P.end_stage()
    P.begin_stage()
    wo = P.sb("OB_wo", [128, 8, D], BF16)
    for k in range(8):
        P.dma("pool", wo[:, k, :], g.c_w_out[g.J(j), k * 128:(k + 1) * 128, :], w=[f"woC{k}"])
    snk = P.sb("OB_snk", [128, 16], F32)
    nsnk = P.sb("OB_nsnk", [128, 16], F32)
    P.dma("sp", snk[:], g.c_sink[g.J(j), :].partition_broadcast(128), w=["snk"])
    P.ts("dve", nsnk[:], snk[:], -1.0, ALU.mult, r=["snk"], w=["nsnk"])
    GG = P.sb("OB_GG", [128, 2, D], F32)
    for s_ in range(2):
        P.dma("sp", GG[:, s_, :], g.modv[l, s_, 2, :].partition_broadcast(128), w=["GG"], stream=f"ogg{s_}")
    qTr = Rot(P, "OB_qT", [128, 8, 128], BF16, 2)
    zr = Rot(P, "OB_z", [128, D], BF16, 2)
    xr = Rot(P, "OB_x", [128, D], F32, 2)
    mxr = Rot(P, "OB_mx", [128, 8], F32, 3)
    prr = Rot(P, "OB_pr", [128, 5, 128], BF16, 2)
    ptsr = Rot(P, "OB_pts", [128, 5, 128], BF16, 2)
    orr = Rot(P, "OB_o", [128, D], F32, 2)
    t1 = P.sb("OB_t1", [128, D], F32)
    t3 = P.sb("OB_t3", [128, D], F32)
    ogr = Rot(P, "OB_og", [128, D], BF16, 2)
    ogTr = Rot(P, "OB_ogT", [128, 8, 128], BF16, 2)
    st4 = Rot(P, "OB_st", [128, 16], F32, 2)
    xor_ = Rot(P, "OB_xo", [128, D], F32, 2)
    p_sc = Rot(P, "OB_psc", [128, 2, 512], F32, 1, psum=True)
    p_pt = Rot(P, "OB_ppt", [128, 5, 128], BF16, 1, psum=True)
    p_oa = Rot(P, "OB_poa", [128, 65], F32, 2, psum=True)
    p_og = Rot(P, "OB_pog", [128, D], BF16, 1, psum=True)
    p_wo = [Rot(P, "OB_pw0", [128, 512], F32, 1, psum=True), Rot(P, "OB_pw1", [128, 512], F32, 1, psum=True)]
    wokeys = [f"woC{k}" for k in range(8)]
    qtiles = list(range(2, NTILE)) + ([] if last else [0, 1])
    import os
    if os.environ.get("KODD") == "A":
        qtiles = []
    _nh = int(os.environ.get("KNH", "16"))
    tog = 0
    for tile in qtiles:
        P.split()
        isctx = tile < 2
        src = 1 if isctx else 0
        tok = tile * 128
        band = [] if isctx else [t for t in (tile - 1, tile, tile + 1) if 2 <= t < NTILE]
        nbt = len(band)
        nkb = 2 + nbt
        qT, qTk = qTr.next()
        P.dma("sp", qT[:].rearrange("p j t -> p (j t)"), g.qt[tile].rearrange("p j t -> p (j t)"), r=["d:qt"], w=[qTk])
        z, zk = zr.next()
        P.dma("sp", z[:], g.zt[tok:tok + 128, :], r=["d:zt"], w=[zk])
        xt, xk = xr.next()
        P.dma("sp", xt[:], xin[tok:tok + 128, :], r=["d:xin"], w=[xk])
        o, ok_ = orr.next()
        for h in range(_nh):
            g_ = h // 4
            pb = (h % 2) * 64
            qtl = h // 2
            sc, sck = p_sc.next()
            P.mm(sc[:, 0, 0:256], qT[pb:pb + 64, qtl, :], kT[pb:pb + 64, g_, 0:256], True, True, r=[qTk], w=[sck])
            if nbt:
                b0 = band[0] * 128
                P.mm(sc[:, 1, 0:nbt * 128], qT[pb:pb + 64, qtl, :], kT[pb:pb + 64, g_, b0:b0 + nbt * 128], True, True, r=[qTk], w=[sck])
            mx, mxk = mxr.next()
            P.op("dve", lambda e, mx=mx, sc=sc: e.tensor_reduce(out=mx[:, 0:1], in_=sc[:, 0, 0:256], axis=AX.X, op=ALU.max), r=[sck], w=[mxk])
            if nbt:
                P.op("dve", lambda e, mx=mx, sc=sc, nbt=nbt: e.tensor_reduce(out=mx[:, 1:2], in_=sc[:, 1, 0:nbt * 128], axis=AX.X, op=ALU.max), r=[sck], w=[mxk])
                P.tt("dve", mx[:, 2:3], mx[:, 0:1], mx[:, 1:2], ALU.max, r=[mxk], w=[mxk])
                msrc = mx[:, 2:3]
            else:
                msrc = mx[:, 0:1]
            P.ts("dve", mx[:, 3:4], msrc, -0.125, ALU.mult, r=[mxk, "nsnk"], w=[mxk], s2=nsnk[:, h:h + 1], op1=ALU.min)
            pr, prk = prr.next()
            P.act(pr[:, 0:2, :], sc[:, 0, 0:256].rearrange("p (b s) -> p b s", b=2), AF.Exp, r=[sck, mxk], w=[prk], bias=mx[:, 3:4], scale=0.125)
            if nbt:
                P.act(pr[:, 2:2 + nbt, :], sc[:, 1, 0:nbt * 128].rearrange("p (b s) -> p b s", b=nbt), AF.Exp, r=[sck, mxk], w=[prk],
                      bias=mx[:, 3:4], scale=0.125)
            P.act(mx[:, 4:5], snk[:, h:h + 1], AF.Exp, r=["snk", mxk], w=[mxk], bias=mx[:, 3:4], scale=1.0)
            for bi, t in enumerate(band):
                if t == tile - 1:
                    P.tt("pool", pr[:, 2 + bi, :], pr[:, 2 + bi, :], g.trifb[:], ALU.mult, r=[prk, "trifb"], w=[prk])
                elif t == tile + 1:
                    P.tt("pool", pr[:, 2 + bi, :], pr[:, 2 + bi, :], g.tribb[:], ALU.mult, r=[prk, "tribb"], w=[prk])
            ptp, ptpk = p_pt.next()
            for b in range(nkb):
                P.tr(ptp[:, b, :], pr[:, b, :], g.ident[:], r=[prk, "ident"], w=[ptpk])
            pts, ptsk = ptsr.next()
            eng = "act" if tog % 2 == 0 else "dve"
            tog += 1
            P.cp(eng, pts[:, 0:nkb, :], ptp[:, 0:nkb, :], r=[ptpk], w=[ptsk])
            oa, oak = p_oa.next()
            for b, kt in enumerate([0, 1] + band):
                P.mm(oa[:], pts[:, b, :], va[:, kt, g_, :], b == 0, b == nkb - 1, r=[ptsk], w=[oak])
            P.tt("dve", mx[:, 5:6], oa[:, 64:65], mx[:, 4:5], ALU.add, r=[oak, mxk], w=[mxk])
            P.recip(mx[:, 6:7], mx[:, 5:6], r=[mxk], w=[mxk])
            P.ts("dve", o[:, h * 64:(h + 1) * 64], oa[:, 0:64], mx[:, 6:7], ALU.mult, r=[oak, mxk], w=[ok_])
        P.act(t1[:], z[:], AF.Exp, r=[zk], w=["t1"], scale=-1.0)
        P.ts("pool", t1[:], t1[:], 1.0, ALU.add, r=["t1"], w=["t1"])
        P.recip(t1[:], t1[:], r=["t1"], w=["t1"])
        P.tt("pool", t1[:], t1[:], z[:], ALU.mult, r=["t1", zk], w=["t1"])
        og, ogk = ogr.next()
        P.tt("pool", og[:], o[:], t1[:], ALU.mult, r=[ok_, "t1"], w=[ogk])
        pg_, pgk_ = p_og.next()
        for k in range(8):
            P.tr(pg_[:, k * 128:(k + 1) * 128], og[:, k * 128:(k + 1) * 128], g.ident[:], r=[ogk, "ident"], w=[pgk_])
        ogT, ogTk = ogTr.next()
        P.cp("act", ogT[:], pg_[:].rearrange("p (k t) -> p k t", k=8), r=[pgk_], w=[ogTk])
        pws = []
        for nt_ in range(2):
            pw_, pwk_ = p_wo[nt_].next()
            for f in range(8):
                P.mm(pw_[:], ogT[:, f, :], wo[:, f, nt_ * 512:(nt_ + 1) * 512], f == 0, f == 7, r=[ogTk, wokeys[f]], w=[pwk_])
            pws.append((pw_, pwk_))
        post_norm_residual(g, pws, xt, xk, GG, src, t3, st4, xor_, xout, tile, last)
    P.end_stage()
    P.end_stage()


def build_program(layers=(0, 1, 2, 3), debug=False, stages=None, single=False):
    P = Prog()
    g = declare_io(P, layers, debug, single)
    load_consts(g)
    xin = g.xc
    for l in layers:
        last = l == 3
        xout = g.xcs[l % 2]
        stage_mod(g, l)
        if l % 2 == 0:
            if stages is None or "A" in stages:
                stage_even_A(g, l, xin)
            if stages is None or "P" in stages:
                stage_even_P(g, l)
            if stages is None or "S" in stages:
                stage_even_S(g, l, xin, xout, last)
        else:
            stage_odd(g, l, xin, xout, last)
        xin = xout
    nc = P.build()
    return P, nc, g


def _const_tables():
    ident = np.eye(128, dtype=np.float32)
    s = np.arange(128)[:, None]
    t = np.arange(128)[None, :]
    trif = (s <= t).astype(np.float32)
    trib = (s >= t).astype(np.float32)
    inv = np.zeros((8, NX + 16), np.float32)
    for gi, wdw in enumerate((2, 4, 8, 16)):
        h = wdw // 2
        for row, n in ((gi, NX), (4 + gi, NCTX)):
            tt = np.arange(n)
            cnt = np.minimum(tt + h, n) - np.maximum(tt - h, 0)
            inv[row, 8:8 + n] = 1.0 / cnt
    rope = np.zeros((NT, 64), np.float32)
    rope[:NCTX, :32] = 1.0
    rows = NX // 64
    row = np.repeat(np.arange(rows), 64).astype(np.float32)
    col = np.tile(np.arange(64), rows).astype(np.float32)
    nf = 16
    invf = (10000.0 ** (-np.arange(nf, dtype=np.float32) / nf)).astype(np.float32)
    ang = np.concatenate([row[:, None] * invf, col[:, None] * invf], -1).astype(np.float32)
    rope[NCTX:, :32] = np.cos(ang)
    rope[NCTX:, 32:] = np.sin(ang)
    return dict(k_ident=ident, k_trif=trif, k_trib=trib, k_invcnt=inv, k_rope=rope)


def make_in_maps(inp, layer=None, xcs=None):
    f = lambda a: np.ascontiguousarray(np.asarray(a, dtype=np.float32))
    c, c_ctx = f(inp["c"]), f(inp["c_ctx"])
    ls = slice(None) if layer is None else slice(layer, layer + 1)
    js = slice(None) if layer is None else slice(layer // 2, layer // 2 + 1)
    even = layer is None or layer % 2 == 0
    odd = layer is None or layer % 2 == 1
    kt = _const_tables()
    shared = dict(w_mod=f(inp["w_mod"][ls]), b_mod=f(inp["b_mod"][ls]), g_pre=f(inp["g_pre"][ls]), g_post=f(inp["g_post"][ls]),
                  k_ident=kt["k_ident"], k_trif=kt["k_trif"], k_trib=kt["k_trib"])
    if even:
        shared.update(
            ab_w_in=f(inp["ab_w_in"][js]), ab_b_gate=f(inp["ab_b_gate"][js]),
            ab_conv=f(np.asarray(inp["ab_conv"]).reshape(2, 3, 16, 128).transpose(0, 3, 2, 1).reshape(2, 128, 48)[js]),
            ab_mnorm=f(inp["ab_mnorm"][js]), ab_pool_w=f(inp["ab_pool_w"][js]),
            ab_pool_scale=f(np.asarray(inp["ab_pool_scale"]).reshape(2, 8, 128).transpose(0, 2, 1)[js]),
            ab_w_out=f(inp["ab_w_out"][js]), k_invcnt=kt["k_invcnt"])
    if odd:
        shared.update(c_w_in=f(inp["c_w_in"][js]), c_sink=f(inp["c_sink"][js]), c_w_out=f(inp["c_w_out"][js]), k_rope=kt["k_rope"])
    maps = []
    for b in range(8):
        m = dict(shared)
        if xcs is None:
            m["xc"] = np.ascontiguousarray(np.concatenate([np.asarray(inp["ctx"][b], np.float32), np.asarray(inp["x"][b], np.float32)], axis=0))
        else:
            m["xc"] = np.ascontiguousarray(xcs[b])
        cs = np.zeros((128, 8, 2), np.float32)
        cs[:, :, 0] = c[b].reshape(8, 128).T
        cs[:, :, 1] = c_ctx.reshape(8, 128).T
        m["cs"] = np.ascontiguousarray(cs.reshape(128, 16))
        maps.append(m)
    return maps


def kernel(**inputs):
    P, nc, g = build_program(layers=(0, 1, 2, 3), debug=())
    maps = make_in_maps(inputs)
    outs = []
    for b in range(0, 8, 2):
        res = run_bass_kernel_spmd(nc, maps[b:b + 2], core_ids=[0, 1])
        for r in res.results:
            outs.append(np.asarray(r["out"], dtype=np.float32))
    return np.stack(outs, axis=0)
```
